# Optimizing a Trainium2 kernel written in Bass

```python
import jax, jax.numpy as jnp
from jax import lax
import numpy as np

D_MODEL = 2048
BATCH = 2
SEQ = 4096
DEPTH = 2

N_MIXERS = 2
N_EVEN = (DEPTH + 1) // 2
N_ODD = DEPTH // 2
PLE_DIM = 256
BLOCK = 128
HEAD_DIM = 128
N_HEADS = D_MODEL // HEAD_DIM
GM_WIDTH = D_MODEL
GM_CHUNK = 128
GM_GROUPS = 16
GM_GROUP_DIM = GM_WIDTH // GM_GROUPS
D_FF = 5632
N_EXPERTS = 8
TOP_K = 2
D_FF_EXPERT = 7168
LN_EPS = 1e-5
ALPHA = (2.0 * DEPTH) ** 0.25
BETA = (8.0 * DEPTH) ** -0.25

kernel_name = "fox_gmlp_moe_deepnorm_hybrid"


def layer_norm(x, g, b):
    xf = x.astype(jnp.float32)
    mu = jnp.mean(xf, axis=-1, keepdims=True)
    var = jnp.mean(jnp.square(xf - mu), axis=-1, keepdims=True)
    y = (xf - mu) * lax.rsqrt(var + LN_EPS) * g.astype(jnp.float32) + b.astype(jnp.float32)
    return y.astype(x.dtype)


def fox_mixer(x, w_in, b_f, w_o):
    B, S, _ = x.shape
    h = x @ w_in
    q, k, v, f = jnp.split(h, [D_MODEL, 2 * D_MODEL, 3 * D_MODEL], axis=-1)
    q = q.reshape(B, S, N_HEADS, HEAD_DIM)
    k = k.reshape(B, S, N_HEADS, HEAD_DIM)
    v = v.reshape(B, S, N_HEADS, HEAD_DIM)
    logf = jax.nn.log_sigmoid((f + b_f).astype(jnp.float32))
    c = jnp.cumsum(logf, axis=1).transpose(0, 2, 1)
    nb = S // BLOCK
    qb = q.reshape(B, nb, BLOCK, N_HEADS, HEAD_DIM).swapaxes(0, 1)
    cb = c.reshape(B, N_HEADS, nb, BLOCK).transpose(2, 0, 1, 3)
    kpos = jnp.arange(S)
    scale = HEAD_DIM ** -0.5

    def one_block(args):
        i, qi, ci = args
        s = jnp.einsum('bqhd,bkhd->bhqk', qi, k).astype(jnp.float32) * scale
        s = s + (ci[..., :, None] - c[:, :, None, :])
        qpos = i * BLOCK + jnp.arange(BLOCK)
        s = jnp.where(kpos[None, :] <= qpos[:, None], s, -jnp.inf)
        pr = jax.nn.softmax(s, axis=-1).astype(v.dtype)
        return jnp.einsum('bhqk,bkhd->bqhd', pr, v)

    o = lax.map(one_block, (jnp.arange(nb), qb, cb))
    o = o.swapaxes(0, 1).reshape(B, S, D_MODEL)
    return o @ w_o


def gmlp_mixer(x, w_in, ln_v_g, ln_v_b, w_s, b_s, w_o):
    B, S, _ = x.shape
    z = jax.nn.gelu(x @ w_in)
    u, v = jnp.split(z, 2, axis=-1)
    v = layer_norm(v, ln_v_g, ln_v_b)
    nc = S // GM_CHUNK
    v = v.reshape(B, nc, GM_CHUNK, GM_GROUPS, GM_GROUP_DIM)
    causal = jnp.tril(jnp.ones((GM_CHUNK, GM_CHUNK), dtype=bool))
    w = jnp.where(causal[None], w_s, 0.0)
    mixed = jnp.einsum('gts,bcsgd->bctgd', w, v) + b_s.T[:, :, None]
    y = u * mixed.reshape(B, S, GM_WIDTH)
    return y @ w_o


def swiglu(x, w_gu, w_down):
    g, u = jnp.split(x @ w_gu, 2, axis=-1)
    return (jax.nn.silu(g) * u) @ w_down


def moe_swiglu(x, w_router, w_gu, w_down):
    B, S, Dm = x.shape
    t = x.reshape(B * S, Dm)
    logits = (t @ w_router).astype(jnp.float32)
    top_val, top_idx = lax.top_k(logits, TOP_K)
    gates = jax.nn.softmax(top_val, axis=-1)
    combine = jnp.sum(jax.nn.one_hot(top_idx, N_EXPERTS, dtype=jnp.float32)
                      * gates[..., None], axis=1)
    y = jnp.zeros_like(t)
    for e in range(N_EXPERTS):
        y = y + combine[:, e:e + 1].astype(t.dtype) * swiglu(t, w_gu[e], w_down[e])
    return y.reshape(B, S, Dm)


def setup_inputs(seed: int = 0) -> dict:
    key = jax.random.key(seed)
    ks = jax.random.split(key, 24)
    D, H, W = D_MODEL, N_HEADS, GM_WIDTH
    nrm = jax.random.normal
    x = nrm(ks[0], (BATCH, SEQ, D), jnp.float32)
    p = nrm(ks[1], (DEPTH, BATCH, SEQ, PLE_DIM), jnp.float32)
    col_scale = jnp.concatenate([jnp.ones((2 * D,)), jnp.full((D,), BETA), jnp.ones((H,))])
    fox_w_in = nrm(ks[2], (N_EVEN, D, 3 * D + H)) * D ** -0.5 * col_scale
    fox_b_f = jax.random.uniform(ks[3], (N_EVEN, H), minval=1.0, maxval=4.0)
    fox_w_o = nrm(ks[4], (N_EVEN, D, D)) * D ** -0.5 * BETA
    gm_w_in = nrm(ks[5], (N_ODD, D, 2 * W)) * D ** -0.5
    gm_ln_v_g = 1.0 + 0.05 * nrm(ks[6], (N_ODD, W))
    gm_ln_v_b = 0.01 * nrm(ks[7], (N_ODD, W))
    gm_w_s = nrm(ks[8], (N_ODD, GM_GROUPS, GM_CHUNK, GM_CHUNK)) * GM_CHUNK ** -0.5
    gm_b_s = 1.0 + 0.1 * nrm(ks[9], (N_ODD, GM_GROUPS, GM_CHUNK))
    gm_w_o = nrm(ks[10], (N_ODD, W, D)) * W ** -0.5 * BETA
    ffn_w_gu = nrm(ks[11], (N_EVEN, D, 2 * D_FF)) * D ** -0.5
    ffn_w_down = nrm(ks[12], (N_EVEN, D_FF, D)) * D_FF ** -0.5 * BETA
    moe_w_router = nrm(ks[13], (N_ODD, D, N_EXPERTS)) * D ** -0.5
    moe_w_gu = nrm(ks[14], (N_ODD, N_EXPERTS, D, 2 * D_FF_EXPERT)) * D ** -0.5
    moe_w_down = nrm(ks[15], (N_ODD, N_EXPERTS, D_FF_EXPERT, D)) * D_FF_EXPERT ** -0.5 * BETA
    ln_mix_g = 1.0 + 0.05 * nrm(ks[16], (DEPTH, D))
    ln_mix_b = 0.01 * nrm(ks[17], (DEPTH, D))
    ln_ch_g = 1.0 + 0.05 * nrm(ks[18], (DEPTH, D))
    ln_ch_b = 0.01 * nrm(ks[19], (DEPTH, D))
    ple_w_proj = nrm(ks[20], (DEPTH, PLE_DIM, D)) * PLE_DIM ** -0.5
    ple_w_gate = nrm(ks[21], (DEPTH, D, D)) * D ** -0.5
    return {"x": x, "p": p,
            "fox_w_in": fox_w_in, "fox_b_f": fox_b_f, "fox_w_o": fox_w_o,
            "gm_w_in": gm_w_in, "gm_ln_v_g": gm_ln_v_g, "gm_ln_v_b": gm_ln_v_b,
            "gm_w_s": gm_w_s, "gm_b_s": gm_b_s, "gm_w_o": gm_w_o,
            "ffn_w_gu": ffn_w_gu, "ffn_w_down": ffn_w_down,
            "moe_w_router": moe_w_router, "moe_w_gu": moe_w_gu, "moe_w_down": moe_w_down,
            "ln_mix_g": ln_mix_g, "ln_mix_b": ln_mix_b, "ln_ch_g": ln_ch_g, "ln_ch_b": ln_ch_b,
            "ple_w_proj": ple_w_proj, "ple_w_gate": ple_w_gate}


def reference(x, p, fox_w_in, fox_b_f, fox_w_o, gm_w_in, gm_ln_v_g, gm_ln_v_b,
              gm_w_s, gm_b_s, gm_w_o, ffn_w_gu, ffn_w_down, moe_w_router, moe_w_gu,
              moe_w_down, ln_mix_g, ln_mix_b, ln_ch_g, ln_ch_b, ple_w_proj, ple_w_gate):
    for i in range(DEPTH):
        j = i // N_MIXERS
        if i % N_MIXERS == 0:
            mix = fox_mixer(x, fox_w_in[j], fox_b_f[j], fox_w_o[j])
        else:
            mix = gmlp_mixer(x, gm_w_in[j], gm_ln_v_g[j], gm_ln_v_b[j],
                             gm_w_s[j], gm_b_s[j], gm_w_o[j])
        x = layer_norm(ALPHA * x + mix, ln_mix_g[i], ln_mix_b[i])
        if i % 2 == 0:
            ch = swiglu(x, ffn_w_gu[i // 2], ffn_w_down[i // 2])
        else:
            ch = moe_swiglu(x, moe_w_router[i // 2], moe_w_gu[i // 2], moe_w_down[i // 2])
        x = layer_norm(ALPHA * x + ch, ln_ch_g[i], ln_ch_b[i])
        x = x + jax.nn.sigmoid(x @ ple_w_gate[i]) * (p[i] @ ple_w_proj[i])
    return x
```

```python
import numpy as np
from contextlib import ExitStack
import concourse.bass as bass
import concourse.mybir as mybir
from concourse.bass_utils import run_bass_kernel_spmd

F32 = mybir.dt.float32
F32R = mybir.dt.float32r
AF = mybir.ActivationFunctionType
ALU = mybir.AluOpType
AX = mybir.AxisListType

ENGS = ("pe", "act", "dve", "pool", "sp")
NEG_BIG = -30000.0


class Cfg:
    def __init__(self, **kw):
        self.D = 2048
        self.B = 2
        self.S = 4096
        self.TC = 1024
        self.DFF = 5632
        self.E = 8
        self.DFFE = 7168
        self.PLE = 256
        self.DEPTH = 2
        self.G = 4
        self.NW = 5
        for k, v in kw.items():
            setattr(self, k, v)
        self.KC = self.D // 128
        self.H = self.KC
        self.NH = self.TC // 512
        self.NSLOT = self.S // 128
        self.NOWN = self.TC // 128
        self.FC = self.DFF // 128
        self.FCE = self.DFFE // 128
        self.PC = self.PLE // 128
        self.CPB = self.S // self.TC
        self.NCORES = self.B * self.CPB
        self.ALPHA = (2.0 * self.DEPTH) ** 0.25
        assert self.FC % self.G == 0 and self.FCE % self.G == 0


class Buf:
    __slots__ = ("name", "w", "r", "dsem")

    def __init__(self, name="", fence=None):
        self.name = name
        self.w = list(fence.items()) if fence else []
        self.r = []
        self.dsem = None


class Prog:
    SEM_LIMIT = 30000

    def __init__(self, nc, stack):
        self.nc = nc
        self.stack = stack
        self.ops = {e: [] for e in ENGS}
        self.cur_sem = {}
        self.cnt = {}
        self.seen = {e: {} for e in ENGS}
        self.nsem = 0
        self.owner = {}
        for e in ENGS:
            self._new_sem(e)

    def _alloc_sem(self, name):
        self.nsem += 1
        return self.stack.enter_context(self.nc.semaphore(name))

    def _new_sem(self, e):
        self.cur_sem[e] = self._alloc_sem(f"s_{e}_{self.nsem}")
        self.cnt[e] = 0
        self.owner[self.cur_sem[e]] = e

    def dma_sem(self, name):
        s = self._alloc_sem(name)
        self.cnt[s] = 0
        return s

    def _collect_waits(self, eng, reads, writes):
        need = {}

        def add(ev):
            s, v = ev
            if need.get(s, 0) < v:
                need[s] = v
        for b in reads:
            for ev in b.w:
                add(ev)
        for b in writes:
            for ev in b.w:
                add(ev)
            for ev in b.r:
                add(ev)
        waits = []
        seen = self.seen[eng]
        for s, v in need.items():
            if eng == "pe" and self.owner.get(s) == "pe":
                continue
            if seen.get(s, 0) < v:
                seen[s] = v
                waits.append((s, v))
        return waits

    def _record(self, ev, reads, writes):
        for b in reads:
            b.r.append(ev)
            if len(b.r) > 64:
                m = {}
                for s, v in b.r:
                    if m.get(s, 0) < v:
                        m[s] = v
                b.r = list(m.items())
        for b in writes:
            b.w = [ev]
            b.r = []

    def op(self, eng, fn, reads=(), writes=(), inc=True):
        if inc and self.cnt[eng] >= self.SEM_LIMIT:
            self._new_sem(eng)
        waits = self._collect_waits(eng, reads, writes)
        sem = self.cur_sem[eng]
        ev = (sem, self.cnt[eng] + 1)
        if inc:
            self.cnt[eng] += 1
        self.ops[eng].append((waits, fn, sem if inc else None, 1))
        self._record(ev, reads, writes)
        return ev

    def dma(self, q, out, in_, sem, reads=(), writes=(), **kw):
        waits = self._collect_waits(q, reads, writes)
        self.cnt[sem] += 16
        ev = (sem, self.cnt[sem])

        def fn(e, out=out, in_=in_, kw=kw):
            return e.dma_start(out=out, in_=in_, **kw)
        self.ops[q].append((waits, fn, sem, 16))
        self._record(ev, reads, writes)
        return ev

    def wait_all(self, eng, bufs):
        waits = self._collect_waits(eng, bufs, bufs)
        self.ops[eng].append((waits, None, None, 0))

    def emit(self):
        nc = self.nc
        ops = self.ops
        with nc.Block() as block:
            def run(e, lst):
                for waits, fn, sem, n in lst:
                    for s, v in waits:
                        e.wait_ge(s, v)
                    if fn is None:
                        continue
                    ins = fn(e)
                    if sem is not None:
                        ins.then_inc(sem, n)

            @block.tensor
            def _(e):
                run(e, ops["pe"])

            @block.scalar
            def _(e):
                run(e, ops["act"])

            @block.vector
            def _(e):
                run(e, ops["dve"])

            @block.gpsimd
            def _(e):
                run(e, ops["pool"])

            @block.sync
            def _(e):
                run(e, ops["sp"])


class SBAlloc:
    BASE = 16512
    LIMIT = 229376 - 256

    def __init__(self, nc):
        self.nc = nc
        self.top = self.BASE
        self.n = 0
        self.fence = {}
        self.scopes = [[]]
        self.peak = self.top
        self.htop = self.LIMIT

    def token(self, name=""):
        b = Buf(name, self.fence)
        self.scopes[-1].append(b)
        return b

    def alloc(self, name, shape, dtype):
        esz = 4 if dtype in (F32, F32R, mybir.dt.int32, mybir.dt.uint32) else 2
        size = esz
        for s in shape[1:]:
            size *= s
        size = (size + 63) // 64 * 64
        self.n += 1
        t = self.nc.alloc_sbuf_tensor_at(f"{name}{self.n}", list(shape), dtype, offset=self.top)
        self.top += size
        assert self.top <= self.htop, f"SBUF overflow at {name}: {self.top} > {self.htop}"
        self.peak = max(self.peak, self.top)
        return t, self.token(name)

    def alloc_high(self, name, shape, dtype):
        size = 4
        for s in shape[1:]:
            size *= s
        size = (size + 63) // 64 * 64
        self.n += 1
        self.htop -= size
        assert self.htop >= self.top, f"SBUF overflow (high) at {name}"
        t = self.nc.alloc_sbuf_tensor_at(f"{name}{self.n}", list(shape), dtype, offset=self.htop)
        b = Buf(name, self.fence)
        return t, b

    def push(self):
        self.scopes.append([])
        return self.top

    def pop(self, mark):
        bufs = self.scopes.pop()
        for b in bufs:
            for s, v in b.w + b.r:
                if self.fence.get(s, 0) < v:
                    self.fence[s] = v
        self.top = mark


def build(cfg):
    nc = bass.Bass("TRN2", target_bir_lowering=False)
    nc.dge_precook = False
    c = cfg
    KC, H, S, TC, NH, NSLOT, NOWN = c.KC, c.H, c.S, c.TC, c.NH, c.NSLOT, c.NOWN
    FC, FCE, PC, E, G = c.FC, c.FCE, c.PC, c.E, c.G
    NBLK = S // 256
    OWN0 = S - TC
    L = c.DEPTH

    def din(name, shape, dt=F32R):
        return nc.dram_tensor(name, list(shape), dt, kind="ExternalInput").ap()

    d_xk = din("xk", [NBLK, 128, KC, 256])
    d_xo = din("xo", [128, KC, TC])
    d_kmask = din("kmask", [128, NSLOT], F32)
    d_pt = din("pt", [L, 128, PC, TC])
    d_wqkv = din("w_qkv", [H, 128, KC, 3, 128])
    d_wf = din("w_f", [128, KC, 64])
    d_bf = din("b_f", [64, 1], F32)
    d_wo = din("w_o", [KC, 128, KC, 128])
    d_wgu = din("w_gu", [FC, 2, 128, KC, 128])
    d_wdn = din("w_dn", [FC, 128, KC, 128])
    d_pleg = din("ple_g", [L, KC, 128, KC, 128])
    d_plep = din("ple_p", [L, KC, 128, PC, 128])
    d_gmin = din("gm_in", [2 * KC, 128, KC, 128])
    d_gmo = din("gm_o", [KC, 128, KC, 128])
    d_gmws = din("gm_ws", [128, KC, 128])
    d_gmbs = din("gm_bs", [1, KC * 128], F32)
    d_gmgb = din("gm_gb", [128, 2, KC], F32)
    d_wr = din("w_r", [128, KC, E], F32)
    d_mgu = din("moe_gu", [E, FCE, 2, 128, KC, 128])
    d_mdn = din("moe_dn", [E, FCE, 128, KC, 128])
    d_lnp = din("ln_par", [128, 4 * L, KC], F32)
    d_sel = din("c_sel", [64, H, 128])
    d_esel = din("c_esel", [8, 8, 128], F32)
    d_identr = din("c_identr", [128, 128])
    d_ident = din("c_ident", [128, 128], F32)
    d_tri = din("c_tri", [128, 128])
    d_c01 = din("c_c01", [128, 128], F32)
    d_out = nc.dram_tensor("out", [128, KC, TC], F32, kind="ExternalOutput").ap()

    with ExitStack() as st:
        p = Prog(nc, st)
        sb = SBAlloc(nc)
        psum = [st.enter_context(nc.psum_tensor(f"ps{i}", [128, 512], F32)) for i in range(8)]
        pbuf = [Buf(f"ps{i}") for i in range(8)]
        state = {"bank": 0, "pool": list(range(8)), "dq": 0}

        def bank():
            pool = state["pool"]
            b = pool[state["bank"] % len(pool)]
            state["bank"] += 1
            return psum[b], pbuf[b]

        def load(out, in_, wbuf, reads=(), q="sp"):
            if wbuf.dsem is None:
                state["dq"] += 1
                wbuf.dsem = p.dma_sem(f"dma{state['dq']}")
            return p.dma(q, out, in_, wbuf.dsem, reads=list(reads), writes=[wbuf])

        def mm(ps, lhsT, rhs, start, stop, reads, pb, inc=None):
            if inc is None:
                inc = stop
            p.op("pe", lambda e: e.matmul(ps, lhsT=lhsT, rhs=rhs, start=start, stop=stop),
                 reads=reads, writes=[pb], inc=inc)

        def act(out, in_, func, reads, writes, bias=None, scale=None):
            kw = {}
            if bias is not None:
                kw["bias"] = bias
            if scale is not None:
                kw["scale"] = scale
            p.op("act", lambda e: e.activation(out=out, in_=in_, func=func, **kw), reads=reads, writes=writes)

        def tt(eng, out, in0, in1, op, reads, writes):
            p.op(eng, lambda e: e.tensor_tensor(out=out, in0=in0, in1=in1, op=op), reads=reads, writes=writes)

        def ts(eng, out, in0, s1, s2, op0, op1, reads, writes):
            if op1 is None:
                p.op(eng, lambda e: e.tensor_scalar(out=out, in0=in0, scalar1=s1, scalar2=None, op0=op0),
                     reads=reads, writes=writes)
            else:
                p.op(eng, lambda e: e.tensor_scalar(out=out, in0=in0, scalar1=s1, scalar2=s2, op0=op0, op1=op1),
                     reads=reads, writes=writes)

        def stt(eng, out, in0, scalar, in1, op0, op1, reads, writes):
            p.op(eng, lambda e: e.scalar_tensor_tensor(out=out, in0=in0, scalar=scalar, in1=in1, op0=op0, op1=op1),
                 reads=reads, writes=writes)

        def copy(eng, out, in_, reads, writes):
            if eng == "act":
                p.op("act", lambda e: e.copy(out=out, in_=in_), reads=reads, writes=writes)
            else:
                p.op(eng, lambda e: e.tensor_copy(out=out, in_=in_), reads=reads, writes=writes)

        rr = {"i": 0}

        def alt():
            rr["i"] += 1
            return "act" if rr["i"] % 2 else "dve"

        IDR, bIDR = sb.alloc("identr", [128, 128], F32R)
        IDF, bIDF = sb.alloc("ident", [128, 128], F32)
        TRI, bTRI = sb.alloc("tri", [128, 128], F32R)
        C01, bC01 = sb.alloc("c01", [128, 128], F32)
        ONES, bONES = sb.alloc("ones", [128, 128], F32R)
        LNP, bLNP = sb.alloc("lnp", [128, 4 * L, KC], F32)
        KMASK, bKMASK = sb.alloc("kmask", [128, NSLOT], F32)
        load(IDR[:], d_identr, bIDR)
        load(IDF[:], d_ident, bIDF)
        load(TRI[:], d_tri, bTRI)
        load(C01[:], d_c01, bC01)
        load(LNP[:], d_lnp, bLNP)
        load(KMASK[:], d_kmask, bKMASK)
        EPS, bEPS = sb.alloc("eps", [128, 1], F32)
        p.op("pool", lambda e: e.memset(EPS[:], 1e-5), writes=[bEPS])
        ONESF, bONESF = sb.alloc("onesf", [128, 128], F32)
        p.op("pool", lambda e: e.memset(ONESF[:], 1.0), writes=[bONESF])
        copy("dve", ONES[:], ONESF[:], [bONESF], [bONES])

        def layer_norm(src, bsrc, src_is_r, dst, bdst, ncols, gi, bi_, gb_tile=None, gb_buf=None):
            m = sb.push()
            MEAN, bMEAN = sb.alloc("mean", [128, 512], F32)
            MSQ, bMSQ = sb.alloc("msq", [128, 512], F32)
            RSTD, bRSTD = sb.alloc("rstd", [128, 512], F32)
            SQ = [sb.alloc("sq", [128, 512], F32R) for _ in range(2)]
            XR = [sb.alloc("xr", [128, 512], F32R) for _ in range(2)] if not src_is_r else None
            T1 = [sb.alloc("t1", [128, 512], F32) for _ in range(2)]
            T2 = [sb.alloc("t2", [128, 512], F32) for _ in range(2)]
            n = ncols
            ps1, pb1 = bank()
            ps2, pb2 = bank()
            for kc in range(KC):
                sq, bsq = SQ[kc % 2]
                act(sq[:, 0:n], src(kc), AF.Square, [bsrc], [bsq])
                if src_is_r:
                    rhs, brhs = src(kc), bsrc
                else:
                    xr, bxr = XR[kc % 2]
                    copy("pool", xr[:, 0:n], src(kc), [bsrc], [bxr])
                    rhs, brhs = xr[:, 0:n], bxr
                mm(ps1[:, 0:n], ONES[:], rhs, kc == 0, kc == KC - 1, [bONES, brhs], pb1, inc=True)
                mm(ps2[:, 0:n], ONES[:], sq[:, 0:n], kc == 0, kc == KC - 1, [bONES, bsq], pb2, inc=True)
            invd = 1.0 / c.D
            ts("dve", MEAN[:, 0:n], ps1[:, 0:n], invd, None, ALU.mult, None, [pb1], [bMEAN])
            tt("dve", MSQ[:, 0:n], MEAN[:, 0:n], MEAN[:, 0:n], ALU.mult, [bMEAN], [bMSQ])
            stt("dve", MSQ[:, 0:n], ps2[:, 0:n], invd, MSQ[:, 0:n], ALU.mult, ALU.subtract, [pb2, bMSQ], [bMSQ])
            act(RSTD[:, 0:n], MSQ[:, 0:n], AF.Sqrt, [bMSQ, bEPS], [bRSTD], bias=EPS[:, 0:1], scale=1.0)
            p.op("dve", lambda e, RSTD=RSTD, n=n: e.reciprocal(out=RSTD[:, 0:n], in_=RSTD[:, 0:n]),
                 reads=[bRSTD], writes=[bRSTD])
            for kc in range(KC):
                t1, bt1 = T1[kc % 2]
                t2, bt2 = T2[kc % 2]
                tt("dve", t1[:, 0:n], src(kc), MEAN[:, 0:n], ALU.subtract, [bsrc, bMEAN], [bt1])
                tt("pool", t2[:, 0:n], t1[:, 0:n], RSTD[:, 0:n], ALU.mult, [bt1, bRSTD], [bt2])
                if gb_tile is None:
                    gcol, bcol, gbb = LNP[:, gi, kc:kc + 1], LNP[:, bi_, kc:kc + 1], bLNP
                else:
                    gcol, bcol, gbb = gb_tile[:, 0, kc:kc + 1], gb_tile[:, 1, kc:kc + 1], gb_buf
                act(dst(kc), t2[:, 0:n], AF.Identity, [bt2, gbb], [bdst], bias=bcol, scale=gcol)
            sb.pop(m)

        def make_slots(n):
            return [sb.alloc_high("wslot", [128, KC, 128], F32R) for _ in range(n)]

        wstate = {"i": 0, "slots": None}

        def wload(src_ap, kdim=None):
            slots = wstate["slots"]
            t, b = slots[wstate["i"] % len(slots)]
            wstate["i"] += 1
            if kdim is None:
                load(t[:], src_ap, b)
            else:
                load(t[:, 0:kdim, :], src_ap, b)
            return t, b

        mOT = sb.push()
        OT, bOT = sb.alloc("oT", [128, KC, TC], F32R)
        mA = sb.push()
        SEL, bSEL = sb.alloc("sel", [64, H, 128], F32R)
        load(SEL[:], d_sel, bSEL)
        CQ, bCQ = sb.alloc("cq", [64, TC], F32R)
        BIASK, bBIASK = sb.alloc("biask", [128, NSLOT, 64], F32)
        XB = [sb.alloc("xb", [128, KC, 256], F32R) for _ in range(2)]

        mA0 = sb.push()
        ZER, bZER = sb.alloc("zer", [64, 512], F32)
        p.op("pool", lambda e: e.memset(ZER[:], 0.0), writes=[bZER])
        WF, bWF = sb.alloc("wf", [128, KC, 64], F32R)
        BF, bBF = sb.alloc("bf", [64, 1], F32)
        NBF, bNBF = sb.alloc("nbf", [64, 1], F32)
        LF, bLF = sb.alloc("lf", [64, S], F32)
        CP, bCP = sb.alloc("cp", [64, S], F32)
        E1 = [sb.alloc("e1", [64, 256], F32) for _ in range(2)]
        load(WF[:], d_wf, bWF)
        load(BF[:], d_bf, bBF)
        ts("dve", NBF[:], BF[:], -1.0, None, ALU.mult, None, [bBF], [bNBF])
        for blk in range(NBLK):
            xb, bxb = XB[blk % 2]
            load(xb[:], d_xk[blk], bxb)
            ps, pb = bank()
            for kc in range(KC):
                mm(ps[0:64, 0:256], WF[:, kc, :], xb[:, kc, :], kc == 0, kc == KC - 1, [bWF, bxb], pb)
            e1, be1 = E1[blk % 2]
            act(e1[:], ps[0:64, 0:256], AF.Exp, [pb, bNBF], [be1], bias=NBF[:, 0:1], scale=-1.0)
            act(LF[:, blk * 256:(blk + 1) * 256], e1[:], AF.Ln, [be1], [bLF], bias=1.0)
        for sblk in range(S // 512):
            s0 = sblk * 512
            init = 0.0 if sblk == 0 else CP[:, s0 - 1:s0]
            p.op("dve", lambda e, s0=s0, init=init: e.tensor_tensor_scan(
                out=CP[:, s0:s0 + 512], data0=LF[:, s0:s0 + 512], data1=ZER[:, 0:512], initial=init,
                op0=ALU.add, op1=ALU.add), reads=[bLF, bZER, bCP], writes=[bCP])
        for slot in range(NSLOT):
            ps, pb = bank()
            p.op("pe", lambda e, ps=ps, slot=slot: e.transpose(
                out=ps[:, 0:64], in_=CP[:, slot * 128:(slot + 1) * 128], identity=IDF[0:64, 0:64]),
                reads=[bCP, bIDF], writes=[pb])
            ts("dve", BIASK[:, slot, :], ps[:, 0:64], KMASK[:, slot:slot + 1], None, ALU.add, None,
               [pb, bKMASK], [bBIASK])
        NEG, bNEG = sb.alloc("neg", [64, TC], F32)
        DIFF, bDIFF = sb.alloc("diff", [64, TC], F32)
        ts("dve", NEG[:], CP[:, OWN0:S], -1.0, None, ALU.mult, None, [bCP], [bNEG])
        copy("dve", CQ[:], NEG[:], [bNEG], [bCQ])
        tt("dve", DIFF[:], NEG[:], CQ[:].bitcast(F32), ALU.subtract, [bNEG, bCQ], [bDIFF])
        copy("dve", CQ[32:64, :], DIFF[32:64, :], [bDIFF], [bCQ])
        sb.pop(mA0)

        WH, bWH = sb.alloc("wh", [128, KC, 3, 128], F32R)
        KT, bKT = sb.alloc("kT", [128, S], F32R)
        V, bV = sb.alloc("v", [128, NSLOT, 128], F32R)
        QT, bQT = sb.alloc("qT", [128, TC], F32R)
        VTT = [sb.alloc("vtt", [128, 256], F32R) for _ in range(2)]
        PT_ = [sb.alloc("pt", [128, 512], F32R) for _ in range(4)]
        RL = [sb.alloc("rl", [128, 512], F32) for _ in range(2)]
        scale = 128.0 ** -0.5
        state["pool"] = [0, 1, 2, 3]
        accb = {"i": 0}
        for h in range(H):
            load(WH[:], d_wqkv[h], bWH)
            for blk in range(NBLK):
                xb, bxb = XB[blk % 2]
                load(xb[:], d_xk[blk], bxb)
                ps, pb = bank()
                for kc in range(KC):
                    mm(ps[:, 0:256], WH[:, kc, 1, :], xb[:, kc, :], kc == 0, kc == KC - 1, [bWH, bxb], pb)
                copy("act", KT[:, blk * 256:(blk + 1) * 256], ps[:, 0:256], [pb], [bKT])
                ps, pb = bank()
                for kc in range(KC):
                    mm(ps[:, 0:256], WH[:, kc, 2, :], xb[:, kc, :], kc == 0, kc == KC - 1, [bWH, bxb], pb)
                vt, bvt = VTT[blk % 2]
                copy("dve", vt[:], ps[:, 0:256], [pb], [bvt])
                for t in range(2):
                    ps, pb = bank()
                    p.op("pe", lambda e, ps=ps, vt=vt, t=t: e.transpose(
                        out=ps[:, 0:128].bitcast(F32R), in_=vt[:, t * 128:(t + 1) * 128], identity=IDR[:]),
                        reads=[bvt, bIDR], writes=[pb])
                    copy(alt(), V[:, blk * 2 + t, :], ps[:, 0:128], [pb], [bV])
                if blk * 256 >= OWN0:
                    off = blk * 256 - OWN0
                    ps, pb = bank()
                    for kc in range(KC):
                        mm(ps[:, 0:256], WH[:, kc, 0, :], xb[:, kc, :], kc == 0, kc == KC - 1, [bWH, bxb], pb)
                    act(QT[:, off:off + 256], ps[:, 0:256], AF.Copy, [pb], [bQT], scale=scale)
            for qt in range(NH):
                tiles = [(s_, None) for s_ in range(NSLOT - NOWN)]
                for i in range(NOWN):
                    if i < 4 * qt:
                        tiles.append((NSLOT - NOWN + i, None))
                    elif i <= 4 * qt + 3:
                        tiles.append((NSLOT - NOWN + i, i - 4 * qt))
                ab = accb["i"] % 2
                accb["i"] += 1
                ps_o, pb_o = psum[4 + 2 * ab], pbuf[4 + 2 * ab]
                ps_l, pb_l = psum[5 + 2 * ab], pbuf[5 + 2 * ab]
                q0 = qt * 512
                nt = len(tiles)
                pend = []

                def score(idx):
                    slot, r = tiles[idx]
                    c0 = 0 if r is None else r * 128
                    ps, pb = bank()
                    mm(ps[:, c0:512], KT[:, slot * 128:(slot + 1) * 128], QT[:, q0 + c0:q0 + 512],
                       True, False, [bKT, bQT], pb, inc=False)
                    if r is not None:
                        mm(ps[:, c0:c0 + 128], IDR[:], TRI[:], False, False, [bIDR, bTRI], pb, inc=False)
                    mm(ps[:, c0:512], SEL[:, h, :], CQ[:, q0 + c0:q0 + 512], False, True, [bSEL, bCQ], pb, inc=True)
                    pt, bpt = PT_[idx % 4]
                    act(pt[:, c0:512], ps[:, c0:512], AF.Exp, [pb, bBIASK], [bpt],
                        bias=BIASK[:, slot, h:h + 1], scale=1.0)
                    return (idx, slot, c0, pt, bpt)

                def pv(item):
                    idx, slot, c0, pt, bpt = item
                    mm(ps_o[:, c0:512], V[:, slot, :], pt[:, c0:512], idx == 0, idx == nt - 1, [bV, bpt], pb_o,
                       inc=(idx == nt - 1))
                    mm(ps_l[:, c0:512], ONES[:], pt[:, c0:512], idx == 0, idx == nt - 1, [bONES, bpt], pb_l,
                       inc=True)

                for idx in range(nt):
                    pend.append(score(idx))
                    if len(pend) > 2:
                        pv(pend.pop(0))
                while pend:
                    pv(pend.pop(0))
                rl, brl = RL[ab]
                p.op("dve", lambda e, rl=rl, ps_l=ps_l: e.reciprocal(out=rl[:], in_=ps_l[:]),
                     reads=[pb_l], writes=[brl])
                tt("dve", OT[:, h, q0:q0 + 512], ps_o[:], rl[:], ALU.mult, [pb_o, brl], [bOT])
        state["pool"] = list(range(8))
        sb.pop(mA)
        mB = sb.push()

        XT, bXT = sb.alloc_high("xT", [128, KC, TC], F32R)
        bXTh = [Buf(f"xTh{i}", sb.fence) for i in range(NH)]
        wstate["slots"] = make_slots(c.NW)

        def xt(kc, hf):
            return XT[:, kc, hf * 512:(hf + 1) * 512]

        load(XT[:], d_xo, bXT)
        for hf in range(NH):
            bXTh[hf].w = list(bXT.w)
        for oc in range(KC):
            w, bw = wload(d_wo[oc])
            banks = [bank() for _ in range(NH)]
            for kc in range(KC):
                for hf in range(NH):
                    ps, pb = banks[hf]
                    mm(ps[:], w[:, kc, :], OT[:, kc, hf * 512:(hf + 1) * 512], kc == 0, kc == KC - 1, [bw, bOT], pb)
            for hf in range(NH):
                ps, pb = banks[hf]
                stt("dve", xt(oc, hf), xt(oc, hf), c.ALPHA, ps[:], ALU.mult, ALU.add, [bXTh[hf], pb], [bXTh[hf]])
        for hf in range(NH):
            layer_norm(lambda kc, hf=hf: xt(kc, hf), bXTh[hf], True, lambda kc, hf=hf: xt(kc, hf), bXTh[hf], 512, 0, 1)
        sb.pop(mB)
        sb.pop(mOT)
        mC = sb.push()

        def ffn_half(hf, experts, ACC, bACC, CB, bCB):
            m = sb.push()
            HG = [sb.alloc("hg", [128, G, 512], F32R) for _ in range(2)]
            SG = [sb.alloc("sg", [128, 512], F32) for _ in range(2)]
            SG2 = [sb.alloc("sg2", [128, 512], F32) for _ in range(2)]
            gi = 0
            ci = 0
            for (e_idx, d_gu_e, d_dn_e, nfc) in experts:
                for g0 in range(0, nfc, G):
                    hg, bhg = HG[gi % 2]
                    gi += 1
                    for j in range(G):
                        fc = g0 + j
                        wg, bwg = wload(d_gu_e[fc, 0])
                        wu, bwu = wload(d_gu_e[fc, 1])
                        psg, pbg = bank()
                        psu, pbu = bank()
                        for kc in range(KC):
                            mm(psg[:], wg[:, kc, :], xt(kc, hf), kc == 0, kc == KC - 1, [bwg, bXTh[hf]], pbg)
                        for kc in range(KC):
                            mm(psu[:], wu[:, kc, :], xt(kc, hf), kc == 0, kc == KC - 1, [bwu, bXTh[hf]], pbu)
                        sg, bsg = SG[ci % 2]
                        act(sg[:], psg[:], AF.Silu, [pbg], [bsg])
                        if CB is not None:
                            sg2, bsg2 = SG2[ci % 2]
                            tt("pool", sg2[:], sg[:], CB[:, e_idx, :], ALU.mult, [bsg, bCB], [bsg2])
                            sg, bsg = sg2, bsg2
                        ci += 1
                        tt("dve", hg[:, j, :], sg[:], psu[:], ALU.mult, [bsg, pbu], [bhg])
                    wds = [wload(d_dn_e[g0 + j]) for j in range(G)]
                    for oc in range(KC):
                        ps, pb = bank()
                        for j in range(G):
                            wd, bwd = wds[j]
                            mm(ps[:], wd[:, oc, :], hg[:, j, :], j == 0, j == G - 1, [bwd, bhg], pb)
                        tt("dve", ACC[:, oc, :], ACC[:, oc, :], ps[:], ALU.add, [bACC, pb], [bACC])
            sb.pop(m)

        def ple_half(layer, hf, last):
            m = sb.push()
            PTL, bPTL = sb.alloc("ptl", [128, PC, 512], F32R)
            XN, bXN = sb.alloc("xn", [128, KC, 512], F32)
            SGT = [sb.alloc("sgt", [128, 512], F32) for _ in range(2)]
            TT_ = [sb.alloc("tt", [128, 512], F32) for _ in range(2)]
            load(PTL[:], d_pt[layer][:, :, hf * 512:(hf + 1) * 512], bPTL)
            for oc in range(KC):
                wg, bwg = wload(d_pleg[layer, oc])
                wp, bwp = wload(d_plep[layer, oc], kdim=PC)
                psg, pbg = bank()
                psp, pbp = bank()
                for kc in range(KC):
                    mm(psg[:], wg[:, kc, :], xt(kc, hf), kc == 0, kc == KC - 1, [bwg, bXTh[hf]], pbg)
                for pc in range(PC):
                    mm(psp[:], wp[:, pc, :], PTL[:, pc, :], pc == 0, pc == PC - 1, [bwp, bPTL], pbp)
                sgt, bsgt = SGT[oc % 2]
                t_, bt_ = TT_[oc % 2]
                act(sgt[:], psg[:], AF.Sigmoid, [pbg], [bsgt])
                tt("dve", t_[:], sgt[:], psp[:], ALU.mult, [bsgt, pbp], [bt_])
                tt("pool", XN[:, oc, :], t_[:], xt(oc, hf).bitcast(F32), ALU.add, [bt_, bXTh[hf]], [bXN])
            if last:
                s = p.dma_sem(f"out{hf}")
                bo = sb.token("outdram")
                p.dma("sp", d_out[:, :, hf * 512:(hf + 1) * 512], XN[:], s, reads=[bXN], writes=[bo])
                outs.append(bo)
            else:
                copy("pool", XT[:, :, hf * 512:(hf + 1) * 512], XN[:], [bXN], [bXTh[hf]])
            sb.pop(m)

        outs = []

        for hf in range(NH):
            m = sb.push()
            ACC, bACC = sb.alloc("acc", [128, KC, 512], F32)
            ts("pool", ACC[:], XT[:, :, hf * 512:(hf + 1) * 512].bitcast(F32), c.ALPHA, None, ALU.mult, None,
               [bXTh[hf]], [bACC])
            ffn_half(hf, [(0, d_wgu, d_wdn, FC)], ACC, bACC, None, None)
            layer_norm(lambda kc: ACC[:, kc, :], bACC, False, lambda kc, hf=hf: xt(kc, hf), bXTh[hf], 512, 2, 3)
            sb.pop(m)
        for hf in range(NH):
            ple_half(0, hf, last=(L == 1))

        if L > 1:
            mE = sb.push()
            WST, bWST = sb.alloc("wst", [128, KC, 128], F32R)
            BSB, bBSB = sb.alloc("bsb", [128, KC, 128], F32)
            GMGB, bGMGB = sb.alloc("gmgb", [128, 2, KC], F32)
            load(WST[:], d_gmws, bWST)
            load(BSB[:].rearrange("p g t -> p (g t)"), d_gmbs.partition_broadcast(128), bBSB)
            load(GMGB[:], d_gmgb, bGMGB)
            for g in range(KC):
                tt("pool", WST[:, g, :], WST[:, g, :].bitcast(F32), C01[:], ALU.mult, [bWST, bC01], [bWST])
            for hf in range(NH):
                m = sb.push()
                U, bU = sb.alloc("u", [128, KC, 512], F32R)
                VT_, bVT = sb.alloc("vT", [128, KC, 512], F32R)
                mg = sb.push()
                X2 = [sb.alloc("x2", [128, 512], F32) for _ in range(2)]
                TG = [sb.alloc("tg", [128, 512], F32) for _ in range(2)]
                SGg = [sb.alloc("sgg", [128, 512], F32) for _ in range(2)]
                for oc in range(2 * KC):
                    w, bw = wload(d_gmin[oc])
                    ps, pb = bank()
                    for kc in range(KC):
                        mm(ps[:], w[:, kc, :], xt(kc, hf), kc == 0, kc == KC - 1, [bw, bXTh[hf]], pb)
                    x2, bx2 = X2[oc % 2]
                    tg, btg = TG[oc % 2]
                    sgg, bsgg = SGg[oc % 2]
                    act(x2[:], ps[:], AF.Square, [pb], [bx2])
                    ts("pool", tg[:], x2[:], 0.044715, 1.0, ALU.mult, ALU.add, [bx2], [btg])
                    tt("dve", tg[:], tg[:], ps[:], ALU.mult, [btg, pb], [btg])
                    act(sgg[:], tg[:], AF.Sigmoid, [btg], [bsgg], scale=1.5957691216057308)
                    dstt, bdst = (U, bU) if oc < KC else (VT_, bVT)
                    tt("dve", dstt[:, oc % KC, :], sgg[:], ps[:], ALU.mult, [bsgg, pb], [bdst])
                sb.pop(mg)
                layer_norm(lambda kc, VT_=VT_: VT_[:, kc, :], bVT, True, lambda kc, VT_=VT_: VT_[:, kc, :], bVT, 512, 0, 1,
                           gb_tile=GMGB, gb_buf=bGMGB)
                mv = sb.push()
                VTM = [sb.alloc("vtm", [128, 128], F32R) for _ in range(4)]
                TM = [sb.alloc("tm", [128, 128], F32) for _ in range(2)]
                k = 0
                for t4 in range(4):
                    for g in range(KC):
                        ps, pb = bank()
                        p.op("pe", lambda e, ps=ps, g=g, t4=t4, VT_=VT_: e.transpose(
                            out=ps[:, 0:128].bitcast(F32R), in_=VT_[:, g, t4 * 128:(t4 + 1) * 128], identity=IDR[:]),
                            reads=[bVT, bIDR], writes=[pb])
                        vtm, bvtm = VTM[k % 4]
                        copy(alt(), vtm[:], ps[:, 0:128], [pb], [bvtm])
                        ps2, pb2 = bank()
                        mm(ps2[:, 0:128], vtm[:], WST[:, g, :], True, True, [bvtm, bWST], pb2)
                        tm, btm = TM[k % 2]
                        tt("dve", tm[:], ps2[:, 0:128], BSB[:, g, :], ALU.add, [pb2, bBSB], [btm])
                        tt("pool", U[:, g, t4 * 128:(t4 + 1) * 128], tm[:],
                           U[:, g, t4 * 128:(t4 + 1) * 128].bitcast(F32), ALU.mult, [btm, bU], [bU])
                        k += 1
                sb.pop(mv)
                for oc in range(KC):
                    w, bw = wload(d_gmo[oc])
                    ps, pb = bank()
                    for kc in range(KC):
                        mm(ps[:], w[:, kc, :], U[:, kc, :], kc == 0, kc == KC - 1, [bw, bU], pb)
                    stt("dve", xt(oc, hf), xt(oc, hf), c.ALPHA, ps[:], ALU.mult, ALU.add, [bXTh[hf], pb], [bXTh[hf]])
                layer_norm(lambda kc, hf=hf: xt(kc, hf), bXTh[hf], True, lambda kc, hf=hf: xt(kc, hf), bXTh[hf],
                           512, 4, 5)
                sb.pop(m)
            sb.pop(mE)

            mF = sb.push()
            WR, bWR = sb.alloc("wr", [128, KC, E], F32)
            load(WR[:], d_wr, bWR)
            ESEL, bESEL = sb.alloc("esel", [8, 8, 128], F32)
            load(ESEL[:], d_esel, bESEL)
            for hf in range(NH):
                m = sb.push()
                ACC, bACC = sb.alloc("acc", [128, KC, 512], F32)
                CB, bCB = sb.alloc("cb", [128, E, 512], F32)
                CMBT, bCMBT = sb.alloc("cmbt", [8, 512], F32)
                m2 = sb.push()
                LG, bLG = sb.alloc("lg", [128, 8], F32)
                MX, bMX = sb.alloc("mx", [128, 8], F32)
                NT1, bNT1 = sb.alloc("nt1", [128, 1], F32)
                MSK, bMSK = sb.alloc("msk", [128, 8], F32)
                EX, bEX = sb.alloc("ex", [128, 8], F32)
                DEN, bDEN = sb.alloc("den", [128, 1], F32)
                CMB, bCMB = sb.alloc("cmb", [128, 8], F32)
                for t4 in range(4):
                    ps, pb = bank()
                    for kc in range(KC):
                        mm(ps[:, 0:E], XT[:, kc, hf * 512 + t4 * 128:hf * 512 + (t4 + 1) * 128].bitcast(F32),
                           WR[:, kc, :], kc == 0, kc == KC - 1, [bXTh[hf], bWR], pb)
                    p.op("pool", lambda e, LG=LG: e.memset(LG[:], -1e30), writes=[bLG])
                    copy("dve", LG[:, 0:E], ps[:, 0:E], [pb], [bLG])
                    p.op("dve", lambda e, MX=MX, LG=LG: e.max(out=MX[:], in_=LG[:]), reads=[bLG], writes=[bMX])
                    ts("dve", NT1[:], MX[:, 0:1], -1.0, None, ALU.mult, None, [bMX], [bNT1])
                    ts("dve", MSK[:], LG[:], MX[:, 1:2], None, ALU.is_ge, None, [bLG, bMX], [bMSK])
                    act(EX[:], LG[:], AF.Exp, [bLG, bNT1], [bEX], bias=NT1[:, 0:1], scale=1.0)
                    tt("dve", EX[:], EX[:], MSK[:], ALU.mult, [bEX, bMSK], [bEX])
                    p.op("dve", lambda e, DEN=DEN, EX=EX: e.reduce_sum(out=DEN[:], in_=EX[:], axis=AX.X), reads=[bEX], writes=[bDEN])
                    p.op("dve", lambda e, DEN=DEN: e.reciprocal(out=DEN[:], in_=DEN[:]), reads=[bDEN], writes=[bDEN])
                    ts("dve", CMB[:], EX[:], DEN[:, 0:1], None, ALU.mult, None, [bEX, bDEN], [bCMB])
                    ps, pb = bank()
                    p.op("pe", lambda e, ps=ps, CMB=CMB: e.transpose(out=ps[0:8, 0:128], in_=CMB[:], identity=IDF[:]),
                         reads=[bCMB, bIDF], writes=[pb])
                    copy("dve", CMBT[:, t4 * 128:(t4 + 1) * 128], ps[0:8, 0:128], [pb], [bCMBT])
                sb.pop(m2)
                for e_ in range(E):
                    ps, pb = bank()
                    mm(ps[:], ESEL[:, e_, :], CMBT[:], True, True, [bESEL, bCMBT], pb)
                    copy("act", CB[:, e_, :], ps[:], [pb], [bCB])
                ts("pool", ACC[:], XT[:, :, hf * 512:(hf + 1) * 512].bitcast(F32), c.ALPHA, None, ALU.mult, None,
                   [bXTh[hf]], [bACC])
                ffn_half(hf, [(e_, d_mgu[e_], d_mdn[e_], FCE) for e_ in range(E)], ACC, bACC, CB, bCB)
                layer_norm(lambda kc: ACC[:, kc, :], bACC, False, lambda kc, hf=hf: xt(kc, hf), bXTh[hf], 512, 6, 7)
                sb.pop(m)
            sb.pop(mF)
            for hf in range(NH):
                ple_half(1, hf, last=True)

        p.wait_all("sp", outs)
        sb.pop(mC)
        p.emit()
        nc._peak_sbuf = sb.peak
    return nc


def _blk(w, kc_n):
    K, N = w.shape
    return np.ascontiguousarray(w.reshape(K // 128, 128, N // 128, 128).transpose(2, 1, 0, 3))


def _rowblk(w):
    K, N = w.shape
    return np.ascontiguousarray(w.reshape(K // 128, 128, N // 128, 128))


def _fm(v, kc_n):
    return np.ascontiguousarray(v.reshape(kc_n, 128).T)


def prep_inputs(cfg, x, p, fox_w_in, fox_b_f, fox_w_o, gm_w_in, gm_ln_v_g, gm_ln_v_b, gm_w_s, gm_b_s, gm_w_o,
                ffn_w_gu, ffn_w_down, moe_w_router, moe_w_gu, moe_w_down, ln_mix_g, ln_mix_b, ln_ch_g, ln_ch_b,
                ple_w_proj, ple_w_gate):
    c = cfg
    D, KC, H, S, TC = c.D, c.KC, c.H, c.S, c.TC
    f32 = np.float32
    x = np.asarray(x, f32)
    p = np.asarray(p, f32)
    shared = {}
    w_in = np.asarray(fox_w_in[0], f32)
    wq, wk, wv, wf = w_in[:, 0:D], w_in[:, D:2 * D], w_in[:, 2 * D:3 * D], w_in[:, 3 * D:3 * D + H]
    qkv = np.stack([_blk(wq, KC), _blk(wk, KC), _blk(wv, KC)], axis=3)
    shared["w_qkv"] = np.ascontiguousarray(qkv)
    wf64 = np.zeros((D, 64), f32)
    wf64[:, 0:H] = wf
    wf64[:, 32:32 + H] = wf
    shared["w_f"] = np.ascontiguousarray(wf64.reshape(KC, 128, 64).transpose(1, 0, 2))
    bf = np.zeros((64, 1), f32)
    bf[0:H, 0] = np.asarray(fox_b_f[0], f32)
    bf[32:32 + H, 0] = np.asarray(fox_b_f[0], f32)
    shared["b_f"] = bf
    shared["w_o"] = _blk(np.asarray(fox_w_o[0], f32), KC)
    gu = np.asarray(ffn_w_gu[0], f32)
    shared["w_gu"] = np.ascontiguousarray(np.stack([_blk(gu[:, :c.DFF], KC), _blk(gu[:, c.DFF:], KC)], axis=1))
    shared["w_dn"] = _rowblk(np.asarray(ffn_w_down[0], f32))
    shared["ple_g"] = np.stack([_blk(np.asarray(ple_w_gate[l], f32), KC) for l in range(c.DEPTH)])
    shared["ple_p"] = np.stack([_blk(np.asarray(ple_w_proj[l], f32), c.PC) for l in range(c.DEPTH)])
    if c.DEPTH > 1:
        shared["gm_in"] = _blk(np.asarray(gm_w_in[0], f32), KC)
        shared["gm_o"] = _blk(np.asarray(gm_w_o[0], f32), KC)
        ws = np.asarray(gm_w_s[0], f32)
        shared["gm_ws"] = np.ascontiguousarray(ws.transpose(2, 0, 1))
        shared["gm_bs"] = np.ascontiguousarray(np.asarray(gm_b_s[0], f32).reshape(1, -1))
        shared["gm_gb"] = np.ascontiguousarray(
            np.stack([_fm(np.asarray(gm_ln_v_g[0], f32), KC), _fm(np.asarray(gm_ln_v_b[0], f32), KC)], axis=1))
        shared["w_r"] = np.ascontiguousarray(
            np.asarray(moe_w_router[0], f32).reshape(KC, 128, c.E).transpose(1, 0, 2))
        mg = np.asarray(moe_w_gu[0], f32)
        shared["moe_gu"] = np.stack(
            [np.stack([_blk(mg[e][:, :c.DFFE], KC), _blk(mg[e][:, c.DFFE:], KC)], axis=1) for e in range(c.E)])
        md = np.asarray(moe_w_down[0], f32)
        shared["moe_dn"] = np.stack([_rowblk(md[e]) for e in range(c.E)])
    else:
        shared["gm_in"] = np.zeros((2 * KC, 128, KC, 128), f32)
        shared["gm_o"] = np.zeros((KC, 128, KC, 128), f32)
        shared["gm_ws"] = np.zeros((128, KC, 128), f32)
        shared["gm_bs"] = np.zeros((1, KC * 128), f32)
        shared["gm_gb"] = np.zeros((128, 2, KC), f32)
        shared["w_r"] = np.zeros((128, KC, c.E), f32)
        shared["moe_gu"] = np.zeros((c.E, c.FCE, 2, 128, KC, 128), f32)
        shared["moe_dn"] = np.zeros((c.E, c.FCE, 128, KC, 128), f32)
    lnp = []
    for l in range(c.DEPTH):
        lnp += [_fm(np.asarray(ln_mix_g[l], f32), KC), _fm(np.asarray(ln_mix_b[l], f32), KC),
                _fm(np.asarray(ln_ch_g[l], f32), KC), _fm(np.asarray(ln_ch_b[l], f32), KC)]
    shared["ln_par"] = np.ascontiguousarray(np.stack(lnp, axis=1))
    sel = np.zeros((64, H, 128), f32)
    for h in range(H):
        sel[h, h, :] = 1.0
        sel[32 + h, h, :] = 1.0
    shared["c_sel"] = sel
    esel = np.zeros((8, 8, 128), f32)
    for e in range(8):
        esel[e, e, :] = 1.0
    shared["c_esel"] = esel
    shared["c_identr"] = np.eye(128, dtype=f32)
    shared["c_ident"] = np.eye(128, dtype=f32)
    kk = np.arange(128)[:, None]
    qq = np.arange(128)[None, :]
    shared["c_tri"] = np.where(kk > qq, NEG_BIG, 0.0).astype(f32)
    shared["c_c01"] = (kk <= qq).astype(f32)

    in_maps = []
    for core in range(c.NCORES):
        b, j = core // c.CPB, core % c.CPB
        m = dict(shared)
        nvalid = (j + 1) * TC
        xk = np.zeros((S, D), f32)
        xk[S - nvalid:] = x[b, 0:nvalid]
        m["xk"] = np.ascontiguousarray(xk.reshape(S // 256, 256, KC, 128).transpose(0, 3, 2, 1))
        xo = x[b, j * TC:(j + 1) * TC]
        m["xo"] = np.ascontiguousarray(xo.reshape(TC, KC, 128).transpose(2, 1, 0))
        km = np.zeros((128, c.NSLOT), f32)
        km[:, 0:(S - nvalid) // 128] = NEG_BIG
        m["kmask"] = km
        pt = p[:, b, j * TC:(j + 1) * TC]
        m["pt"] = np.ascontiguousarray(pt.reshape(c.DEPTH, TC, c.PC, 128).transpose(0, 3, 2, 1))
        in_maps.append(m)
    return in_maps


def assemble(cfg, results):
    c = cfg
    out = np.zeros((c.B, c.S, c.D), np.float32)
    for core in range(c.NCORES):
        b, j = core // c.CPB, core % c.CPB
        o = results[core]["out"]
        out[b, j * c.TC:(j + 1) * c.TC] = o.transpose(2, 1, 0).reshape(c.TC, c.D)
    return out


_NC_CACHE = {}


def kernel(**inputs):
    cfg = Cfg()
    if "nc" not in _NC_CACHE:
        _NC_CACHE["nc"] = build(cfg)
    nc = _NC_CACHE["nc"]
    in_maps = prep_inputs(cfg, **inputs)
    res = run_bass_kernel_spmd(nc, in_maps, core_ids=list(range(cfg.NCORES)))
    return assemble(cfg, res.results)
```

```python
import numpy as np
from contextlib import ExitStack
import concourse.bass as bass
import concourse.mybir as mybir
from concourse.bass_utils import run_bass_kernel_spmd

F32 = mybir.dt.float32
F32R = mybir.dt.float32r
AF = mybir.ActivationFunctionType
ALU = mybir.AluOpType
AX = mybir.AxisListType

ENGS = ("pe", "act", "dve", "pool", "sp")
NEG_BIG = -30000.0


class Cfg:
    def __init__(self, **kw):
        self.D = 2048
        self.B = 2
        self.S = 4096
        self.TC = 1024
        self.DFF = 5632
        self.E = 8
        self.DFFE = 7168
        self.PLE = 256
        self.DEPTH = 2
        self.G = 4
        self.NW = 5
        for k, v in kw.items():
            setattr(self, k, v)
        self.KC = self.D // 128
        self.H = self.KC
        self.NH = self.TC // 512
        self.NSLOT = self.S // 128
        self.NOWN = self.TC // 128
        self.FC = self.DFF // 128
        self.FCE = self.DFFE // 128
        self.PC = self.PLE // 128
        self.CPB = self.S // self.TC
        self.NCORES = self.B * self.CPB
        self.ALPHA = (2.0 * self.DEPTH) ** 0.25
        assert self.FC % self.G == 0 and self.FCE % self.G == 0


class Buf:
    __slots__ = ("name", "w", "r", "dsem")

    def __init__(self, name="", fence=None):
        self.name = name
        self.w = list(fence.items()) if fence else []
        self.r = []
        self.dsem = None


class Prog:
    SEM_LIMIT = 30000

    def __init__(self, nc, stack):
        self.nc = nc
        self.stack = stack
        self.ops = {e: [] for e in ENGS}
        self.cur_sem = {}
        self.cnt = {}
        self.seen = {e: {} for e in ENGS}
        self.nsem = 0
        self.owner = {}
        for e in ENGS:
            self._new_sem(e)

    def _alloc_sem(self, name):
        self.nsem += 1
        return self.stack.enter_context(self.nc.semaphore(name))

    def _new_sem(self, e):
        self.cur_sem[e] = self._alloc_sem(f"s_{e}_{self.nsem}")
        self.cnt[e] = 0
        self.owner[self.cur_sem[e]] = e

    def dma_sem(self, name):
        s = self._alloc_sem(name)
        self.cnt[s] = 0
        return s

    def _collect_waits(self, eng, reads, writes):
        need = {}

        def add(ev):
            s, v = ev
            if need.get(s, 0) < v:
                need[s] = v
        for b in reads:
            for ev in b.w:
                add(ev)
        for b in writes:
            for ev in b.w:
                add(ev)
            for ev in b.r:
                add(ev)
        waits = []
        seen = self.seen[eng]
        for s, v in need.items():
            if eng == "pe" and self.owner.get(s) == "pe":
                continue
            if seen.get(s, 0) < v:
                seen[s] = v
                waits.append((s, v))
        return waits

    def _record(self, ev, reads, writes):
        for b in reads:
            b.r.append(ev)
            if len(b.r) > 64:
                m = {}
                for s, v in b.r:
                    if m.get(s, 0) < v:
                        m[s] = v
                b.r = list(m.items())
        for b in writes:
            b.w = [ev]
            b.r = []

    def op(self, eng, fn, reads=(), writes=(), inc=True):
        if inc and self.cnt[eng] >= self.SEM_LIMIT:
            self._new_sem(eng)
        waits = self._collect_waits(eng, reads, writes)
        sem = self.cur_sem[eng]
        ev = (sem, self.cnt[eng] + 1)
        if inc:
            self.cnt[eng] += 1
        self.ops[eng].append((waits, fn, sem if inc else None, 1))
        self._record(ev, reads, writes)
        return ev

    def dma(self, q, out, in_, sem, reads=(), writes=(), **kw):
        waits = self._collect_waits(q, reads, writes)
        self.cnt[sem] += 16
        ev = (sem, self.cnt[sem])

        def fn(e, out=out, in_=in_, kw=kw):
            return e.dma_start(out=out, in_=in_, **kw)
        self.ops[q].append((waits, fn, sem, 16))
        self._record(ev, reads, writes)
        return ev

    def wait_all(self, eng, bufs):
        waits = self._collect_waits(eng, bufs, bufs)
        self.ops[eng].append((waits, None, None, 0))

    def emit(self):
        nc = self.nc
        ops = self.ops
        with nc.Block() as block:
            def run(e, lst):
                for waits, fn, sem, n in lst:
                    for s, v in waits:
                        e.wait_ge(s, v)
                    if fn is None:
                        continue
                    ins = fn(e)
                    if sem is not None:
                        ins.then_inc(sem, n)

            @block.tensor
            def _(e):
                run(e, ops["pe"])

            @block.scalar
            def _(e):
                run(e, ops["act"])

            @block.vector
            def _(e):
                run(e, ops["dve"])

            @block.gpsimd
            def _(e):
                run(e, ops["pool"])

            @block.sync
            def _(e):
                run(e, ops["sp"])


class SBAlloc:
    BASE = 16512
    LIMIT = 229376 - 256

    def __init__(self, nc):
        self.nc = nc
        self.top = self.BASE
        self.n = 0
        self.fence = {}
        self.scopes = [[]]
        self.peak = self.top
        self.htop = self.LIMIT
        self.freed = []
        self.ranges = {}

    def token(self, name=""):
        b = Buf(name, self.fence)
        self.scopes[-1].append(b)
        return b

    def _inherit(self, lo, hi):
        ev = dict(self.fence)
        for (a, b_, e) in self.freed:
            if a < hi and lo < b_:
                for s, v in e.items():
                    if ev.get(s, 0) < v:
                        ev[s] = v
        return ev

    def alloc(self, name, shape, dtype):
        esz = 4 if dtype in (F32, F32R, mybir.dt.int32, mybir.dt.uint32) else 2
        size = esz
        for s in shape[1:]:
            size *= s
        size = (size + 63) // 64 * 64
        self.n += 1
        t = self.nc.alloc_sbuf_tensor_at(f"{name}{self.n}", list(shape), dtype, offset=self.top)
        lo, hi = self.top, self.top + size
        self.top += size
        assert self.top <= self.htop, f"SBUF overflow at {name}: {self.top} > {self.htop}"
        self.peak = max(self.peak, self.top)
        b = Buf(name, self._inherit(lo, hi))
        self.ranges[b] = (lo, hi)
        self.scopes[-1].append(b)
        return t, b

    def alloc_high(self, name, shape, dtype):
        size = 4
        for s in shape[1:]:
            size *= s
        size = (size + 63) // 64 * 64
        self.n += 1
        self.htop -= size
        assert self.htop >= self.top, f"SBUF overflow (high) at {name}"
        t = self.nc.alloc_sbuf_tensor_at(f"{name}{self.n}", list(shape), dtype, offset=self.htop)
        b = Buf(name, self._inherit(self.htop, self.htop + size))
        return t, b

    def push(self):
        self.scopes.append([])
        return self.top

    def pop(self, mark):
        bufs = self.scopes.pop()
        for b in bufs:
            if b in self.ranges:
                ev = {}
                for s, v in b.w + b.r:
                    if ev.get(s, 0) < v:
                        ev[s] = v
                lo, hi = self.ranges.pop(b)
                self.freed.append((lo, hi, ev))
            else:
                for s, v in b.w + b.r:
                    if self.fence.get(s, 0) < v:
                        self.fence[s] = v
        self.top = mark


def build(cfg):
    nc = bass.Bass("TRN2", target_bir_lowering=False)
    nc.dge_precook = False
    c = cfg
    KC, H, S, TC, NH, NSLOT, NOWN = c.KC, c.H, c.S, c.TC, c.NH, c.NSLOT, c.NOWN
    FC, FCE, PC, E, G = c.FC, c.FCE, c.PC, c.E, c.G
    NBLK = S // 256
    OWN0 = S - TC
    L = c.DEPTH

    def din(name, shape, dt=F32R):
        return nc.dram_tensor(name, list(shape), dt, kind="ExternalInput").ap()

    d_xk = din("xk", [NBLK, 128, KC, 256])
    d_xo = din("xo", [128, KC, TC])
    d_kmask = din("kmask", [128, NSLOT], F32)
    d_pt = din("pt", [L, 128, PC, TC])
    d_wqkv = din("w_qkv", [H, 128, KC, 3, 128])
    d_wf = din("w_f", [128, KC, 64])
    d_bf = din("b_f", [64, 1], F32)
    d_wo = din("w_o", [KC, 128, KC, 128])
    d_wgu = din("w_gu", [FC, 2, 128, KC, 128])
    d_wdn = din("w_dn", [FC, 128, KC, 128])
    d_pleg = din("ple_g", [L, KC, 128, KC, 128])
    d_plep = din("ple_p", [L, KC, 128, PC, 128])
    d_gmin = din("gm_in", [2 * KC, 128, KC, 128])
    d_gmo = din("gm_o", [KC, 128, KC, 128])
    d_gmws = din("gm_ws", [128, KC, 128])
    d_gmbs = din("gm_bs", [1, KC * 128], F32)
    d_gmgb = din("gm_gb", [128, 2, KC], F32)
    d_wr = din("w_r", [128, KC, E], F32)
    d_mgu = din("moe_gu", [E, FCE, 2, 128, KC, 128])
    d_mdn = din("moe_dn", [E, FCE, 128, KC, 128])
    d_lnp = din("ln_par", [128, 4 * L, KC], F32)
    d_sel = din("c_sel", [64, H, 128])
    d_esel = din("c_esel", [8, 8, 128], F32)
    d_identr = din("c_identr", [128, 128])
    d_ident = din("c_ident", [128, 128], F32)
    d_tri = din("c_tri", [128, 128])
    d_c01 = din("c_c01", [128, 128], F32)
    d_out = nc.dram_tensor("out", [128, KC, TC], F32, kind="ExternalOutput").ap()

    with ExitStack() as st:
        p = Prog(nc, st)
        sb = SBAlloc(nc)
        psum = [st.enter_context(nc.psum_tensor(f"ps{i}", [128, 512], F32)) for i in range(8)]
        pbuf = [Buf(f"ps{i}") for i in range(8)]
        state = {"bank": 0, "pool": list(range(8)), "dq": 0}

        def bank():
            pool = state["pool"]
            b = pool[state["bank"] % len(pool)]
            state["bank"] += 1
            return psum[b], pbuf[b]

        def load(out, in_, wbuf, reads=(), q="sp"):
            if wbuf.dsem is None:
                state["dq"] += 1
                wbuf.dsem = p.dma_sem(f"dma{state['dq']}")
            return p.dma(q, out, in_, wbuf.dsem, reads=list(reads), writes=[wbuf])

        def mm(ps, lhsT, rhs, start, stop, reads, pb, inc=None):
            if inc is None:
                inc = stop
            p.op("pe", lambda e: e.matmul(ps, lhsT=lhsT, rhs=rhs, start=start, stop=stop),
                 reads=reads, writes=[pb], inc=inc)

        def act(out, in_, func, reads, writes, bias=None, scale=None):
            kw = {}
            if bias is not None:
                kw["bias"] = bias
            if scale is not None:
                kw["scale"] = scale
            p.op("act", lambda e: e.activation(out=out, in_=in_, func=func, **kw), reads=reads, writes=writes)

        def tt(eng, out, in0, in1, op, reads, writes):
            p.op(eng, lambda e: e.tensor_tensor(out=out, in0=in0, in1=in1, op=op), reads=reads, writes=writes)

        def ts(eng, out, in0, s1, s2, op0, op1, reads, writes):
            if op1 is None:
                p.op(eng, lambda e: e.tensor_scalar(out=out, in0=in0, scalar1=s1, scalar2=None, op0=op0),
                     reads=reads, writes=writes)
            else:
                p.op(eng, lambda e: e.tensor_scalar(out=out, in0=in0, scalar1=s1, scalar2=s2, op0=op0, op1=op1),
                     reads=reads, writes=writes)

        def stt(eng, out, in0, scalar, in1, op0, op1, reads, writes):
            p.op(eng, lambda e: e.scalar_tensor_tensor(out=out, in0=in0, scalar=scalar, in1=in1, op0=op0, op1=op1),
                 reads=reads, writes=writes)

        def copy(eng, out, in_, reads, writes):
            if eng == "act":
                p.op("act", lambda e: e.copy(out=out, in_=in_), reads=reads, writes=writes)
            else:
                p.op(eng, lambda e: e.tensor_copy(out=out, in_=in_), reads=reads, writes=writes)

        rr = {"i": 0}

        def alt():
            rr["i"] += 1
            return "act" if rr["i"] % 2 else "dve"

        IDR, bIDR = sb.alloc("identr", [128, 128], F32R)
        IDF, bIDF = sb.alloc("ident", [128, 128], F32)
        TRI, bTRI = sb.alloc("tri", [128, 128], F32R)
        C01, bC01 = sb.alloc("c01", [128, 128], F32)
        ONES, bONES = sb.alloc("ones", [128, 128], F32R)
        LNP, bLNP = sb.alloc("lnp", [128, 4 * L, KC], F32)
        KMASK, bKMASK = sb.alloc("kmask", [128, NSLOT], F32)
        load(IDR[:], d_identr, bIDR)
        load(IDF[:], d_ident, bIDF)
        load(TRI[:], d_tri, bTRI)
        load(C01[:], d_c01, bC01)
        load(LNP[:], d_lnp, bLNP)
        load(KMASK[:], d_kmask, bKMASK)
        EPS, bEPS = sb.alloc("eps", [128, 1], F32)
        p.op("pool", lambda e: e.memset(EPS[:], 1e-5), writes=[bEPS])
        ONESF, bONESF = sb.alloc("onesf", [128, 128], F32)
        p.op("pool", lambda e: e.memset(ONESF[:], 1.0), writes=[bONESF])
        copy("dve", ONES[:], ONESF[:], [bONESF], [bONES])

        def layer_norm(src, bsrc, src_is_r, dst, bdst, ncols, gi, bi_, gb_tile=None, gb_buf=None):
            m = sb.push()
            MEAN, bMEAN = sb.alloc("mean", [128, 512], F32)
            MSQ, bMSQ = sb.alloc("msq", [128, 512], F32)
            RSTD, bRSTD = sb.alloc("rstd", [128, 512], F32)
            SQ = [sb.alloc("sq", [128, 512], F32R) for _ in range(2)]
            XR = [sb.alloc("xr", [128, 512], F32R) for _ in range(2)] if not src_is_r else None
            T1 = [sb.alloc("t1", [128, 512], F32) for _ in range(2)]
            T2 = [sb.alloc("t2", [128, 512], F32) for _ in range(2)]
            n = ncols
            ps1, pb1 = bank()
            ps2, pb2 = bank()
            for kc in range(KC):
                sq, bsq = SQ[kc % 2]
                act(sq[:, 0:n], src(kc), AF.Square, [bsrc], [bsq])
                if src_is_r:
                    rhs, brhs = src(kc), bsrc
                else:
                    xr, bxr = XR[kc % 2]
                    copy("pool", xr[:, 0:n], src(kc), [bsrc], [bxr])
                    rhs, brhs = xr[:, 0:n], bxr
                mm(ps1[:, 0:n], ONES[:], rhs, kc == 0, kc == KC - 1, [bONES, brhs], pb1, inc=True)
                mm(ps2[:, 0:n], ONES[:], sq[:, 0:n], kc == 0, kc == KC - 1, [bONES, bsq], pb2, inc=True)
            invd = 1.0 / c.D
            ts("dve", MEAN[:, 0:n], ps1[:, 0:n], invd, None, ALU.mult, None, [pb1], [bMEAN])
            tt("dve", MSQ[:, 0:n], MEAN[:, 0:n], MEAN[:, 0:n], ALU.mult, [bMEAN], [bMSQ])
            stt("dve", MSQ[:, 0:n], ps2[:, 0:n], invd, MSQ[:, 0:n], ALU.mult, ALU.subtract, [pb2, bMSQ], [bMSQ])
            act(RSTD[:, 0:n], MSQ[:, 0:n], AF.Sqrt, [bMSQ, bEPS], [bRSTD], bias=EPS[:, 0:1], scale=1.0)
            p.op("dve", lambda e, RSTD=RSTD, n=n: e.reciprocal(out=RSTD[:, 0:n], in_=RSTD[:, 0:n]),
                 reads=[bRSTD], writes=[bRSTD])
            for kc in range(KC):
                t1, bt1 = T1[kc % 2]
                t2, bt2 = T2[kc % 2]
                tt("dve", t1[:, 0:n], src(kc), MEAN[:, 0:n], ALU.subtract, [bsrc, bMEAN], [bt1])
                tt("pool", t2[:, 0:n], t1[:, 0:n], RSTD[:, 0:n], ALU.mult, [bt1, bRSTD], [bt2])
                if gb_tile is None:
                    gcol, bcol, gbb = LNP[:, gi, kc:kc + 1], LNP[:, bi_, kc:kc + 1], bLNP
                else:
                    gcol, bcol, gbb = gb_tile[:, 0, kc:kc + 1], gb_tile[:, 1, kc:kc + 1], gb_buf
                act(dst(kc), t2[:, 0:n], AF.Identity, [bt2, gbb], [bdst], bias=bcol, scale=gcol)
            sb.pop(m)

        def make_slots(n):
            return [sb.alloc_high("wslot", [128, KC, 128], F32R) for _ in range(n)]

        wstate = {"i": 0, "slots": None}

        def wload(src_ap, kdim=None):
            slots = wstate["slots"]
            t, b = slots[wstate["i"] % len(slots)]
            wstate["i"] += 1
            if kdim is None:
                load(t[:], src_ap, b)
            else:
                load(t[:, 0:kdim, :], src_ap, b)
            return t, b

        mOT = sb.push()
        OT, bOT = sb.alloc("oT", [128, KC, TC], F32R)
        mA = sb.push()
        SEL, bSEL = sb.alloc("sel", [64, H, 128], F32R)
        load(SEL[:], d_sel, bSEL)
        CQ, bCQ = sb.alloc("cq", [64, TC], F32R)
        BIASK, bBIASK = sb.alloc("biask", [128, NSLOT, 64], F32)
        XB = [sb.alloc("xb", [128, KC, 256], F32R) for _ in range(2)]

        mA0 = sb.push()
        ZER, bZER = sb.alloc("zer", [64, 512], F32)
        p.op("pool", lambda e: e.memset(ZER[:], 0.0), writes=[bZER])
        WF, bWF = sb.alloc("wf", [128, KC, 64], F32R)
        BF, bBF = sb.alloc("bf", [64, 1], F32)
        NBF, bNBF = sb.alloc("nbf", [64, 1], F32)
        LF, bLF = sb.alloc("lf", [64, S], F32)
        CP, bCP = sb.alloc("cp", [64, S], F32)
        E1 = [sb.alloc("e1", [64, 256], F32) for _ in range(2)]
        load(WF[:], d_wf, bWF)
        load(BF[:], d_bf, bBF)
        ts("dve", NBF[:], BF[:], -1.0, None, ALU.mult, None, [bBF], [bNBF])
        for blk in range(NBLK):
            xb, bxb = XB[blk % 2]
            load(xb[:], d_xk[blk], bxb)
            ps, pb = bank()
            for kc in range(KC):
                mm(ps[0:64, 0:256], WF[:, kc, :], xb[:, kc, :], kc == 0, kc == KC - 1, [bWF, bxb], pb)
            e1, be1 = E1[blk % 2]
            act(e1[:], ps[0:64, 0:256], AF.Exp, [pb, bNBF], [be1], bias=NBF[:, 0:1], scale=-1.0)
            act(LF[:, blk * 256:(blk + 1) * 256], e1[:], AF.Ln, [be1], [bLF], bias=1.0)
        for sblk in range(S // 512):
            s0 = sblk * 512
            init = 0.0 if sblk == 0 else CP[:, s0 - 1:s0]
            p.op("dve", lambda e, s0=s0, init=init: e.tensor_tensor_scan(
                out=CP[:, s0:s0 + 512], data0=LF[:, s0:s0 + 512], data1=ZER[:, 0:512], initial=init,
                op0=ALU.add, op1=ALU.add), reads=[bLF, bZER, bCP], writes=[bCP])
        for slot in range(NSLOT):
            ps, pb = bank()
            p.op("pe", lambda e, ps=ps, slot=slot: e.transpose(
                out=ps[:, 0:64], in_=CP[:, slot * 128:(slot + 1) * 128], identity=IDF[0:64, 0:64]),
                reads=[bCP, bIDF], writes=[pb])
            ts("dve", BIASK[:, slot, :], ps[:, 0:64], KMASK[:, slot:slot + 1], None, ALU.add, None,
               [pb, bKMASK], [bBIASK])
        NEG, bNEG = sb.alloc("neg", [64, TC], F32)
        DIFF, bDIFF = sb.alloc("diff", [64, TC], F32)
        ts("dve", NEG[:], CP[:, OWN0:S], -1.0, None, ALU.mult, None, [bCP], [bNEG])
        copy("dve", CQ[:], NEG[:], [bNEG], [bCQ])
        tt("dve", DIFF[:], NEG[:], CQ[:].bitcast(F32), ALU.subtract, [bNEG, bCQ], [bDIFF])
        copy("dve", CQ[32:64, :], DIFF[32:64, :], [bDIFF], [bCQ])
        sb.pop(mA0)

        WH, bWH = sb.alloc("wh", [128, KC, 3, 128], F32R)
        KT, bKT = sb.alloc("kT", [128, S], F32R)
        V, bV = sb.alloc("v", [128, NSLOT, 128], F32R)
        QT, bQT = sb.alloc("qT", [128, TC], F32R)
        VTT = [sb.alloc("vtt", [128, 256], F32R) for _ in range(2)]
        PT_ = [sb.alloc("pt", [128, 512], F32R) for _ in range(4)]
        RL = [sb.alloc("rl", [128, 512], F32) for _ in range(2)]
        scale = 128.0 ** -0.5
        state["pool"] = [0, 1, 2, 3]
        accb = {"i": 0}
        for h in range(H):
            load(WH[:], d_wqkv[h], bWH)
            for blk in range(NBLK):
                xb, bxb = XB[blk % 2]
                load(xb[:], d_xk[blk], bxb)
                ps, pb = bank()
                for kc in range(KC):
                    mm(ps[:, 0:256], WH[:, kc, 1, :], xb[:, kc, :], kc == 0, kc == KC - 1, [bWH, bxb], pb)
                copy("act", KT[:, blk * 256:(blk + 1) * 256], ps[:, 0:256], [pb], [bKT])
                ps, pb = bank()
                for kc in range(KC):
                    mm(ps[:, 0:256], WH[:, kc, 2, :], xb[:, kc, :], kc == 0, kc == KC - 1, [bWH, bxb], pb)
                vt, bvt = VTT[blk % 2]
                copy("dve", vt[:], ps[:, 0:256], [pb], [bvt])
                for t in range(2):
                    ps, pb = bank()
                    p.op("pe", lambda e, ps=ps, vt=vt, t=t: e.transpose(
                        out=ps[:, 0:128].bitcast(F32R), in_=vt[:, t * 128:(t + 1) * 128], identity=IDR[:]),
                        reads=[bvt, bIDR], writes=[pb])
                    copy(alt(), V[:, blk * 2 + t, :], ps[:, 0:128], [pb], [bV])
                if blk * 256 >= OWN0:
                    off = blk * 256 - OWN0
                    ps, pb = bank()
                    for kc in range(KC):
                        mm(ps[:, 0:256], WH[:, kc, 0, :], xb[:, kc, :], kc == 0, kc == KC - 1, [bWH, bxb], pb)
                    act(QT[:, off:off + 256], ps[:, 0:256], AF.Copy, [pb], [bQT], scale=scale)
            for qt in range(NH):
                tiles = [(s_, None) for s_ in range(NSLOT - NOWN)]
                for i in range(NOWN):
                    if i < 4 * qt:
                        tiles.append((NSLOT - NOWN + i, None))
                    elif i <= 4 * qt + 3:
                        tiles.append((NSLOT - NOWN + i, i - 4 * qt))
                ab = accb["i"] % 2
                accb["i"] += 1
                ps_o, pb_o = psum[4 + 2 * ab], pbuf[4 + 2 * ab]
                ps_l, pb_l = psum[5 + 2 * ab], pbuf[5 + 2 * ab]
                q0 = qt * 512
                nt = len(tiles)
                pend = []

                def score(idx):
                    slot, r = tiles[idx]
                    c0 = 0 if r is None else r * 128
                    ps, pb = bank()
                    mm(ps[:, c0:512], KT[:, slot * 128:(slot + 1) * 128], QT[:, q0 + c0:q0 + 512],
                       True, False, [bKT, bQT], pb, inc=False)
                    if r is not None:
                        mm(ps[:, c0:c0 + 128], IDR[:], TRI[:], False, False, [bIDR, bTRI], pb, inc=False)
                    mm(ps[:, c0:512], SEL[:, h, :], CQ[:, q0 + c0:q0 + 512], False, True, [bSEL, bCQ], pb, inc=True)
                    pt, bpt = PT_[idx % 4]
                    act(pt[:, c0:512], ps[:, c0:512], AF.Exp, [pb, bBIASK], [bpt],
                        bias=BIASK[:, slot, h:h + 1], scale=1.0)
                    return (idx, slot, c0, pt, bpt)

                def pv(item):
                    idx, slot, c0, pt, bpt = item
                    mm(ps_o[:, c0:512], V[:, slot, :], pt[:, c0:512], idx == 0, idx == nt - 1, [bV, bpt], pb_o,
                       inc=(idx == nt - 1))
                    mm(ps_l[:, c0:512], ONES[:], pt[:, c0:512], idx == 0, idx == nt - 1, [bONES, bpt], pb_l,
                       inc=True)

                for idx in range(nt):
                    pend.append(score(idx))
                    if len(pend) > 2:
                        pv(pend.pop(0))
                while pend:
                    pv(pend.pop(0))
                rl, brl = RL[ab]
                p.op("dve", lambda e, rl=rl, ps_l=ps_l: e.reciprocal(out=rl[:], in_=ps_l[:]),
                     reads=[pb_l], writes=[brl])
                tt("dve", OT[:, h, q0:q0 + 512], ps_o[:], rl[:], ALU.mult, [pb_o, brl], [bOT])
        state["pool"] = list(range(8))
        sb.pop(mA)
        mB = sb.push()

        XT, bXT = sb.alloc_high("xT", [128, KC, TC], F32R)
        bXTh = [Buf(f"xTh{i}", dict(bXT.w)) for i in range(NH)]
        wstate["slots"] = make_slots(c.NW)

        def xt(kc, hf):
            return XT[:, kc, hf * 512:(hf + 1) * 512]

        load(XT[:], d_xo, bXT)
        for hf in range(NH):
            bXTh[hf].w = list(bXT.w)
        for oc in range(KC):
            w, bw = wload(d_wo[oc])
            banks = [bank() for _ in range(NH)]
            for kc in range(KC):
                for hf in range(NH):
                    ps, pb = banks[hf]
                    mm(ps[:], w[:, kc, :], OT[:, kc, hf * 512:(hf + 1) * 512], kc == 0, kc == KC - 1, [bw, bOT], pb)
            for hf in range(NH):
                ps, pb = banks[hf]
                stt("dve", xt(oc, hf), xt(oc, hf), c.ALPHA, ps[:], ALU.mult, ALU.add, [bXTh[hf], pb], [bXTh[hf]])
        for hf in range(NH):
            layer_norm(lambda kc, hf=hf: xt(kc, hf), bXTh[hf], True, lambda kc, hf=hf: xt(kc, hf), bXTh[hf], 512, 0, 1)
        sb.pop(mB)
        sb.pop(mOT)
        mC = sb.push()

        def ffn_half(hf, experts, ACC, bACC, CB, bCB):
            m = sb.push()
            HG = [sb.alloc("hg", [128, G, 512], F32R) for _ in range(2)]
            SG = [sb.alloc("sg", [128, 512], F32) for _ in range(2)]
            SG2 = [sb.alloc("sg2", [128, 512], F32) for _ in range(2)]
            gi = 0
            ci = 0
            pending = [None]

            def make_down(hg, bhg, d_dn_e, g0):
                def down():
                    wds = [wload(d_dn_e[g0 + j]) for j in range(G)]
                    for oc in range(KC):
                        ps, pb = bank()
                        for j in range(G):
                            wd, bwd = wds[j]
                            mm(ps[:], wd[:, oc, :], hg[:, j, :], j == 0, j == G - 1, [bwd, bhg], pb)
                        tt("dve", ACC[:, oc, :], ACC[:, oc, :], ps[:], ALU.add, [bACC, pb], [bACC])
                return down

            for (e_idx, d_gu_e, d_dn_e, nfc) in experts:
                for g0 in range(0, nfc, G):
                    hg, bhg = HG[gi % 2]
                    gi += 1
                    for j in range(G):
                        fc = g0 + j
                        wg, bwg = wload(d_gu_e[fc, 0])
                        wu, bwu = wload(d_gu_e[fc, 1])
                        psg, pbg = bank()
                        psu, pbu = bank()
                        for kc in range(KC):
                            mm(psg[:], wg[:, kc, :], xt(kc, hf), kc == 0, kc == KC - 1, [bwg, bXTh[hf]], pbg)
                        for kc in range(KC):
                            mm(psu[:], wu[:, kc, :], xt(kc, hf), kc == 0, kc == KC - 1, [bwu, bXTh[hf]], pbu)
                        sg, bsg = SG[ci % 2]
                        act(sg[:], psg[:], AF.Silu, [pbg], [bsg])
                        if CB is not None:
                            sg2, bsg2 = SG2[ci % 2]
                            tt("pool", sg2[:], sg[:], CB[:, e_idx, :], ALU.mult, [bsg, bCB], [bsg2])
                            sg, bsg = sg2, bsg2
                        ci += 1
                        tt("dve", hg[:, j, :], sg[:], psu[:], ALU.mult, [bsg, pbu], [bhg])
                        if j == 0 and pending[0] is not None:
                            pending[0]()
                            pending[0] = None
                    pending[0] = make_down(hg, bhg, d_dn_e, g0)
            if pending[0] is not None:
                pending[0]()
            sb.pop(m)

        def ple_half(layer, hf, last):
            m = sb.push()
            PTL, bPTL = sb.alloc("ptl", [128, PC, 512], F32R)
            XN, bXN = sb.alloc("xn", [128, KC, 512], F32)
            SGT = [sb.alloc("sgt", [128, 512], F32) for _ in range(2)]
            TT_ = [sb.alloc("tt", [128, 512], F32) for _ in range(2)]
            load(PTL[:], d_pt[layer][:, :, hf * 512:(hf + 1) * 512], bPTL)
            for oc in range(KC):
                wg, bwg = wload(d_pleg[layer, oc])
                wp, bwp = wload(d_plep[layer, oc], kdim=PC)
                psg, pbg = bank()
                psp, pbp = bank()
                for kc in range(KC):
                    mm(psg[:], wg[:, kc, :], xt(kc, hf), kc == 0, kc == KC - 1, [bwg, bXTh[hf]], pbg)
                for pc in range(PC):
                    mm(psp[:], wp[:, pc, :], PTL[:, pc, :], pc == 0, pc == PC - 1, [bwp, bPTL], pbp)
                sgt, bsgt = SGT[oc % 2]
                t_, bt_ = TT_[oc % 2]
                act(sgt[:], psg[:], AF.Sigmoid, [pbg], [bsgt])
                tt("dve", t_[:], sgt[:], psp[:], ALU.mult, [bsgt, pbp], [bt_])
                tt("pool", XN[:, oc, :], t_[:], xt(oc, hf).bitcast(F32), ALU.add, [bt_, bXTh[hf]], [bXN])
            if last:
                s = p.dma_sem(f"out{hf}")
                bo = sb.token("outdram")
                p.dma("sp", d_out[:, :, hf * 512:(hf + 1) * 512], XN[:], s, reads=[bXN], writes=[bo])
                outs.append(bo)
            else:
                copy("pool", XT[:, :, hf * 512:(hf + 1) * 512], XN[:], [bXN], [bXTh[hf]])
            sb.pop(m)

        outs = []

        for hf in range(NH):
            m = sb.push()
            ACC, bACC = sb.alloc("acc", [128, KC, 512], F32)
            ts("pool", ACC[:], XT[:, :, hf * 512:(hf + 1) * 512].bitcast(F32), c.ALPHA, None, ALU.mult, None,
               [bXTh[hf]], [bACC])
            ffn_half(hf, [(0, d_wgu, d_wdn, FC)], ACC, bACC, None, None)
            layer_norm(lambda kc: ACC[:, kc, :], bACC, False, lambda kc, hf=hf: xt(kc, hf), bXTh[hf], 512, 2, 3)
            sb.pop(m)
        for hf in range(NH):
            ple_half(0, hf, last=(L == 1))

        if L > 1:
            mE = sb.push()
            WST, bWST = sb.alloc("wst", [128, KC, 128], F32R)
            BSB, bBSB = sb.alloc("bsb", [128, KC, 128], F32)
            GMGB, bGMGB = sb.alloc("gmgb", [128, 2, KC], F32)
            load(WST[:], d_gmws, bWST)
            load(BSB[:].rearrange("p g t -> p (g t)"), d_gmbs.partition_broadcast(128), bBSB)
            load(GMGB[:], d_gmgb, bGMGB)
            for g in range(KC):
                tt("pool", WST[:, g, :], WST[:, g, :].bitcast(F32), C01[:], ALU.mult, [bWST, bC01], [bWST])
            for hf in range(NH):
                m = sb.push()
                U, bU = sb.alloc("u", [128, KC, 512], F32R)
                VT_, bVT = sb.alloc("vT", [128, KC, 512], F32R)
                mg = sb.push()
                X2 = [sb.alloc("x2", [128, 512], F32) for _ in range(2)]
                TG = [sb.alloc("tg", [128, 512], F32) for _ in range(2)]
                SGg = [sb.alloc("sgg", [128, 512], F32) for _ in range(2)]
                for oc in range(2 * KC):
                    w, bw = wload(d_gmin[oc])
                    ps, pb = bank()
                    for kc in range(KC):
                        mm(ps[:], w[:, kc, :], xt(kc, hf), kc == 0, kc == KC - 1, [bw, bXTh[hf]], pb)
                    x2, bx2 = X2[oc % 2]
                    tg, btg = TG[oc % 2]
                    sgg, bsgg = SGg[oc % 2]
                    act(x2[:], ps[:], AF.Square, [pb], [bx2])
                    ts("pool", tg[:], x2[:], 0.044715, 1.0, ALU.mult, ALU.add, [bx2], [btg])
                    tt("dve", tg[:], tg[:], ps[:], ALU.mult, [btg, pb], [btg])
                    act(sgg[:], tg[:], AF.Sigmoid, [btg], [bsgg], scale=1.5957691216057308)
                    dstt, bdst = (U, bU) if oc < KC else (VT_, bVT)
                    tt("dve", dstt[:, oc % KC, :], sgg[:], ps[:], ALU.mult, [bsgg, pb], [bdst])
                sb.pop(mg)
                layer_norm(lambda kc, VT_=VT_: VT_[:, kc, :], bVT, True, lambda kc, VT_=VT_: VT_[:, kc, :], bVT, 512, 0, 1,
                           gb_tile=GMGB, gb_buf=bGMGB)
                mv = sb.push()
                VTM = [sb.alloc("vtm", [128, 128], F32R) for _ in range(4)]
                TM = [sb.alloc("tm", [128, 128], F32) for _ in range(2)]
                k = 0
                for t4 in range(4):
                    for g in range(KC):
                        ps, pb = bank()
                        p.op("pe", lambda e, ps=ps, g=g, t4=t4, VT_=VT_: e.transpose(
                            out=ps[:, 0:128].bitcast(F32R), in_=VT_[:, g, t4 * 128:(t4 + 1) * 128], identity=IDR[:]),
                            reads=[bVT, bIDR], writes=[pb])
                        vtm, bvtm = VTM[k % 4]
                        copy(alt(), vtm[:], ps[:, 0:128], [pb], [bvtm])
                        ps2, pb2 = bank()
                        mm(ps2[:, 0:128], vtm[:], WST[:, g, :], True, True, [bvtm, bWST], pb2)
                        tm, btm = TM[k % 2]
                        tt("dve", tm[:], ps2[:, 0:128], BSB[:, g, :], ALU.add, [pb2, bBSB], [btm])
                        tt("pool", U[:, g, t4 * 128:(t4 + 1) * 128], tm[:],
                           U[:, g, t4 * 128:(t4 + 1) * 128].bitcast(F32), ALU.mult, [btm, bU], [bU])
                        k += 1
                sb.pop(mv)
                for oc in range(KC):
                    w, bw = wload(d_gmo[oc])
                    ps, pb = bank()
                    for kc in range(KC):
                        mm(ps[:], w[:, kc, :], U[:, kc, :], kc == 0, kc == KC - 1, [bw, bU], pb)
                    stt("dve", xt(oc, hf), xt(oc, hf), c.ALPHA, ps[:], ALU.mult, ALU.add, [bXTh[hf], pb], [bXTh[hf]])
                layer_norm(lambda kc, hf=hf: xt(kc, hf), bXTh[hf], True, lambda kc, hf=hf: xt(kc, hf), bXTh[hf],
                           512, 4, 5)
                sb.pop(m)
            sb.pop(mE)

            mF = sb.push()
            WR, bWR = sb.alloc("wr", [128, KC, E], F32)
            load(WR[:], d_wr, bWR)
            ESEL, bESEL = sb.alloc("esel", [8, 8, 128], F32)
            load(ESEL[:], d_esel, bESEL)
            for hf in range(NH):
                m = sb.push()
                ACC, bACC = sb.alloc("acc", [128, KC, 512], F32)
                CB, bCB = sb.alloc("cb", [128, E, 512], F32)
                CMBT, bCMBT = sb.alloc("cmbt", [8, 512], F32)
                m2 = sb.push()
                LG, bLG = sb.alloc("lg", [128, 8], F32)
                MX, bMX = sb.alloc("mx", [128, 8], F32)
                NT1, bNT1 = sb.alloc("nt1", [128, 1], F32)
                MSK, bMSK = sb.alloc("msk", [128, 8], F32)
                EX, bEX = sb.alloc("ex", [128, 8], F32)
                DEN, bDEN = sb.alloc("den", [128, 1], F32)
                CMB, bCMB = sb.alloc("cmb", [128, 8], F32)
                for t4 in range(4):
                    ps, pb = bank()
                    for kc in range(KC):
                        mm(ps[:, 0:E], XT[:, kc, hf * 512 + t4 * 128:hf * 512 + (t4 + 1) * 128].bitcast(F32),
                           WR[:, kc, :], kc == 0, kc == KC - 1, [bXTh[hf], bWR], pb)
                    p.op("pool", lambda e, LG=LG: e.memset(LG[:], -1e30), writes=[bLG])
                    copy("dve", LG[:, 0:E], ps[:, 0:E], [pb], [bLG])
                    p.op("dve", lambda e, MX=MX, LG=LG: e.max(out=MX[:], in_=LG[:]), reads=[bLG], writes=[bMX])
                    ts("dve", NT1[:], MX[:, 0:1], -1.0, None, ALU.mult, None, [bMX], [bNT1])
                    ts("dve", MSK[:], LG[:], MX[:, 1:2], None, ALU.is_ge, None, [bLG, bMX], [bMSK])
                    act(EX[:], LG[:], AF.Exp, [bLG, bNT1], [bEX], bias=NT1[:, 0:1], scale=1.0)
                    tt("dve", EX[:], EX[:], MSK[:], ALU.mult, [bEX, bMSK], [bEX])
                    p.op("dve", lambda e, DEN=DEN, EX=EX: e.reduce_sum(out=DEN[:], in_=EX[:], axis=AX.X), reads=[bEX], writes=[bDEN])
                    p.op("dve", lambda e, DEN=DEN: e.reciprocal(out=DEN[:], in_=DEN[:]), reads=[bDEN], writes=[bDEN])
                    ts("dve", CMB[:], EX[:], DEN[:, 0:1], None, ALU.mult, None, [bEX, bDEN], [bCMB])
                    ps, pb = bank()
                    p.op("pe", lambda e, ps=ps, CMB=CMB: e.transpose(out=ps[0:8, 0:128], in_=CMB[:], identity=IDF[:]),
                         reads=[bCMB, bIDF], writes=[pb])
                    copy("dve", CMBT[:, t4 * 128:(t4 + 1) * 128], ps[0:8, 0:128], [pb], [bCMBT])
                sb.pop(m2)
                for e_ in range(E):
                    ps, pb = bank()
                    mm(ps[:], ESEL[:, e_, :], CMBT[:], True, True, [bESEL, bCMBT], pb)
                    copy("act", CB[:, e_, :], ps[:], [pb], [bCB])
                ts("pool", ACC[:], XT[:, :, hf * 512:(hf + 1) * 512].bitcast(F32), c.ALPHA, None, ALU.mult, None,
                   [bXTh[hf]], [bACC])
                ffn_half(hf, [(e_, d_mgu[e_], d_mdn[e_], FCE) for e_ in range(E)], ACC, bACC, CB, bCB)
                layer_norm(lambda kc: ACC[:, kc, :], bACC, False, lambda kc, hf=hf: xt(kc, hf), bXTh[hf], 512, 6, 7)
                sb.pop(m)
            sb.pop(mF)
            for hf in range(NH):
                ple_half(1, hf, last=True)

        p.wait_all("sp", outs)
        sb.pop(mC)
        p.emit()
        nc._peak_sbuf = sb.peak
    return nc


def _blk(w, kc_n):
    K, N = w.shape
    return np.ascontiguousarray(w.reshape(K // 128, 128, N // 128, 128).transpose(2, 1, 0, 3))


def _rowblk(w):
    K, N = w.shape
    return np.ascontiguousarray(w.reshape(K // 128, 128, N // 128, 128))


def _fm(v, kc_n):
    return np.ascontiguousarray(v.reshape(kc_n, 128).T)


def prep_inputs(cfg, x, p, fox_w_in, fox_b_f, fox_w_o, gm_w_in, gm_ln_v_g, gm_ln_v_b, gm_w_s, gm_b_s, gm_w_o,
                ffn_w_gu, ffn_w_down, moe_w_router, moe_w_gu, moe_w_down, ln_mix_g, ln_mix_b, ln_ch_g, ln_ch_b,
                ple_w_proj, ple_w_gate):
    c = cfg
    D, KC, H, S, TC = c.D, c.KC, c.H, c.S, c.TC
    f32 = np.float32
    x = np.asarray(x, f32)
    p = np.asarray(p, f32)
    shared = {}
    w_in = np.asarray(fox_w_in[0], f32)
    wq, wk, wv, wf = w_in[:, 0:D], w_in[:, D:2 * D], w_in[:, 2 * D:3 * D], w_in[:, 3 * D:3 * D + H]
    qkv = np.stack([_blk(wq, KC), _blk(wk, KC), _blk(wv, KC)], axis=3)
    shared["w_qkv"] = np.ascontiguousarray(qkv)
    wf64 = np.zeros((D, 64), f32)
    wf64[:, 0:H] = wf
    wf64[:, 32:32 + H] = wf
    shared["w_f"] = np.ascontiguousarray(wf64.reshape(KC, 128, 64).transpose(1, 0, 2))
    bf = np.zeros((64, 1), f32)
    bf[0:H, 0] = np.asarray(fox_b_f[0], f32)
    bf[32:32 + H, 0] = np.asarray(fox_b_f[0], f32)
    shared["b_f"] = bf
    shared["w_o"] = _blk(np.asarray(fox_w_o[0], f32), KC)
    gu = np.asarray(ffn_w_gu[0], f32)
    shared["w_gu"] = np.ascontiguousarray(np.stack([_blk(gu[:, :c.DFF], KC), _blk(gu[:, c.DFF:], KC)], axis=1))
    shared["w_dn"] = _rowblk(np.asarray(ffn_w_down[0], f32))
    shared["ple_g"] = np.stack([_blk(np.asarray(ple_w_gate[l], f32), KC) for l in range(c.DEPTH)])
    shared["ple_p"] = np.stack([_blk(np.asarray(ple_w_proj[l], f32), c.PC) for l in range(c.DEPTH)])
    if c.DEPTH > 1:
        shared["gm_in"] = _blk(np.asarray(gm_w_in[0], f32), KC)
        shared["gm_o"] = _blk(np.asarray(gm_w_o[0], f32), KC)
        ws = np.asarray(gm_w_s[0], f32)
        shared["gm_ws"] = np.ascontiguousarray(ws.transpose(2, 0, 1))
        shared["gm_bs"] = np.ascontiguousarray(np.asarray(gm_b_s[0], f32).reshape(1, -1))
        shared["gm_gb"] = np.ascontiguousarray(
            np.stack([_fm(np.asarray(gm_ln_v_g[0], f32), KC), _fm(np.asarray(gm_ln_v_b[0], f32), KC)], axis=1))
        shared["w_r"] = np.ascontiguousarray(
            np.asarray(moe_w_router[0], f32).reshape(KC, 128, c.E).transpose(1, 0, 2))
        mg = np.asarray(moe_w_gu[0], f32)
        shared["moe_gu"] = np.stack(
            [np.stack([_blk(mg[e][:, :c.DFFE], KC), _blk(mg[e][:, c.DFFE:], KC)], axis=1) for e in range(c.E)])
        md = np.asarray(moe_w_down[0], f32)
        shared["moe_dn"] = np.stack([_rowblk(md[e]) for e in range(c.E)])
    else:
        shared["gm_in"] = np.zeros((2 * KC, 128, KC, 128), f32)
        shared["gm_o"] = np.zeros((KC, 128, KC, 128), f32)
        shared["gm_ws"] = np.zeros((128, KC, 128), f32)
        shared["gm_bs"] = np.zeros((1, KC * 128), f32)
        shared["gm_gb"] = np.zeros((128, 2, KC), f32)
        shared["w_r"] = np.zeros((128, KC, c.E), f32)
        shared["moe_gu"] = np.zeros((c.E, c.FCE, 2, 128, KC, 128), f32)
        shared["moe_dn"] = np.zeros((c.E, c.FCE, 128, KC, 128), f32)
    lnp = []
    for l in range(c.DEPTH):
        lnp += [_fm(np.asarray(ln_mix_g[l], f32), KC), _fm(np.asarray(ln_mix_b[l], f32), KC),
                _fm(np.asarray(ln_ch_g[l], f32), KC), _fm(np.asarray(ln_ch_b[l], f32), KC)]
    shared["ln_par"] = np.ascontiguousarray(np.stack(lnp, axis=1))
    sel = np.zeros((64, H, 128), f32)
    for h in range(H):
        sel[h, h, :] = 1.0
        sel[32 + h, h, :] = 1.0
    shared["c_sel"] = sel
    esel = np.zeros((8, 8, 128), f32)
    for e in range(8):
        esel[e, e, :] = 1.0
    shared["c_esel"] = esel
    shared["c_identr"] = np.eye(128, dtype=f32)
    shared["c_ident"] = np.eye(128, dtype=f32)
    kk = np.arange(128)[:, None]
    qq = np.arange(128)[None, :]
    shared["c_tri"] = np.where(kk > qq, NEG_BIG, 0.0).astype(f32)
    shared["c_c01"] = (kk <= qq).astype(f32)

    in_maps = []
    for core in range(c.NCORES):
        b, j = core // c.CPB, core % c.CPB
        m = dict(shared)
        nvalid = (j + 1) * TC
        xk = np.zeros((S, D), f32)
        xk[S - nvalid:] = x[b, 0:nvalid]
        m["xk"] = np.ascontiguousarray(xk.reshape(S // 256, 256, KC, 128).transpose(0, 3, 2, 1))
        xo = x[b, j * TC:(j + 1) * TC]
        m["xo"] = np.ascontiguousarray(xo.reshape(TC, KC, 128).transpose(2, 1, 0))
        km = np.zeros((128, c.NSLOT), f32)
        km[:, 0:(S - nvalid) // 128] = NEG_BIG
        m["kmask"] = km
        pt = p[:, b, j * TC:(j + 1) * TC]
        m["pt"] = np.ascontiguousarray(pt.reshape(c.DEPTH, TC, c.PC, 128).transpose(0, 3, 2, 1))
        in_maps.append(m)
    return in_maps


def assemble(cfg, results):
    c = cfg
    out = np.zeros((c.B, c.S, c.D), np.float32)
    for core in range(c.NCORES):
        b, j = core // c.CPB, core % c.CPB
        o = results[core]["out"]
        out[b, j * c.TC:(j + 1) * c.TC] = o.transpose(2, 1, 0).reshape(c.TC, c.D)
    return out


_NC_CACHE = {}


def kernel(**inputs):
    cfg = Cfg()
    if "nc" not in _NC_CACHE:
        _NC_CACHE["nc"] = build(cfg)
    nc = _NC_CACHE["nc"]
    in_maps = prep_inputs(cfg, **inputs)
    res = run_bass_kernel_spmd(nc, in_maps, core_ids=list(range(cfg.NCORES)))
    return assemble(cfg, res.results)
```
